# Optimizing a Trainium2 kernel written in Bass

```python
import jax, jax.numpy as jnp
from jax import lax
import numpy as np

D_MODEL = 1024
BATCH = 8
SEQ = 4096
DEPTH = 4

D_FF = 2816
NORM_EPS = 1e-6
POOL_WINDOWS = (2, 4, 8, 16)
POOL_GROUP_DIM = D_MODEL // 8
POOL_GROUPS = len(POOL_WINDOWS)
POOL_WIDTH = POOL_GROUPS * POOL_GROUP_DIM
SGU_GROUPS = 4
SGU_GROUP_DIM = D_MODEL // 8
SGU_WIDTH = SGU_GROUPS * SGU_GROUP_DIM
SGU_CHUNK = 128
AB_IN_WIDTH = POOL_WIDTH + 2 * SGU_WIDTH
AB_OUT_WIDTH = POOL_WIDTH + SGU_WIDTH
N_HEADS = 8
HEAD_DIM = D_MODEL // N_HEADS
ROT_DIM = HEAD_DIM // 4
ROPE_THETA = 500000.0
MOBA_BLOCK = 256
MOBA_TOPK = 3
QUERY_CHUNK = 16
NEG_INF = -1e30
N_EVEN = (DEPTH + 1) // 2
N_ODD = DEPTH // 2

kernel_name = 'hybrid_pool_sgu_moba_macaron'


def rms_norm(x, g):
    xf = x.astype(jnp.float32)
    y = xf * lax.rsqrt(jnp.mean(xf * xf, axis=-1, keepdims=True) + NORM_EPS)
    return (y * g.astype(jnp.float32)).astype(x.dtype)


def swiglu(h, w_gate, w_up, w_down):
    return (jax.nn.silu(h @ w_gate) * (h @ w_up)) @ w_down


def pool_mixer(a, w, scale):
    S = a.shape[1]
    af = a.astype(jnp.float32)
    cs0 = jnp.pad(jnp.cumsum(af, axis=1), ((0, 0), (1, 0), (0, 0), (0, 0)))
    t = jnp.arange(S)
    pooled = []
    for g, win in enumerate(POOL_WINDOWS):
        lo_idx = jnp.maximum(t + 1 - win, 0)
        win_sum = cs0[:, 1:, g] - cs0[:, lo_idx, g]
        count = jnp.minimum(t + 1, win).astype(jnp.float32)
        pooled.append(win_sum / count[None, :, None])
    d = (jnp.stack(pooled, axis=2) - af).astype(a.dtype)
    return jnp.einsum('bsgc,gcd->bsgd', d, w) * scale


def sgu_mixer(u, v, norm_g, w_s, b_s):
    B, S, G, C = u.shape
    u = jax.nn.gelu(u, approximate=False)
    v = rms_norm(jax.nn.gelu(v, approximate=False), norm_g)
    vc = v.reshape(B, S // SGU_CHUNK, SGU_CHUNK, G, C)
    causal = jnp.tril(jnp.ones((SGU_CHUNK, SGU_CHUNK), dtype=bool))
    w = jnp.where(causal[None], w_s, jnp.zeros_like(w_s))
    s = jnp.einsum('gts,bnsgc->bntgc', w, vc) + jnp.transpose(b_s)[:, :, None]
    return u * s.reshape(B, S, G, C)


def pool_sgu_mixer(h, w_in, pool_w, pool_scale, sgu_norm, sgu_w, sgu_b, w_out):
    B, S, _ = h.shape
    z = h @ w_in
    a = z[..., :POOL_WIDTH].reshape(B, S, POOL_GROUPS, POOL_GROUP_DIM)
    u = z[..., POOL_WIDTH:POOL_WIDTH + SGU_WIDTH].reshape(B, S, SGU_GROUPS, SGU_GROUP_DIM)
    v = z[..., POOL_WIDTH + SGU_WIDTH:].reshape(B, S, SGU_GROUPS, SGU_GROUP_DIM)
    y_a = pool_mixer(a, pool_w, pool_scale).reshape(B, S, POOL_WIDTH)
    y_b = sgu_mixer(u, v, sgu_norm, sgu_w, sgu_b).reshape(B, S, SGU_WIDTH)
    return jnp.concatenate([y_a, y_b], axis=-1) @ w_out


def partial_rotary(x, cos, sin):
    half = ROT_DIM // 2
    xf = x[..., :ROT_DIM].astype(jnp.float32)
    x1, x2 = xf[..., :half], xf[..., half:]
    rot = jnp.concatenate([x1 * cos - x2 * sin, x2 * cos + x1 * sin], axis=-1).astype(x.dtype)
    return jnp.concatenate([rot, x[..., ROT_DIM:]], axis=-1)


def moba_mixer(h, w_qkv, w_o):
    B, S, _ = h.shape
    qkv = (h @ w_qkv).reshape(B, S, 3, N_HEADS, HEAD_DIM).transpose(2, 0, 3, 1, 4)
    q, k, v = qkv[0], qkv[1], qkv[2]
    pos = jnp.arange(S, dtype=jnp.float32)
    inv_freq = 1.0 / (ROPE_THETA ** (jnp.arange(0, ROT_DIM, 2, dtype=jnp.float32) / ROT_DIM))
    ang = pos[:, None] * inv_freq[None, :]
    cos, sin = jnp.cos(ang), jnp.sin(ang)
    q = partial_rotary(q, cos, sin)
    k = partial_rotary(k, cos, sin)
    n_blocks = -(-S // MOBA_BLOCK)
    pad = n_blocks * MOBA_BLOCK - S
    kb = jnp.pad(k, ((0, 0), (0, 0), (0, pad), (0, 0))).reshape(B, N_HEADS, n_blocks, MOBA_BLOCK, HEAD_DIM)
    vb = jnp.pad(v, ((0, 0), (0, 0), (0, pad), (0, 0))).reshape(B, N_HEADS, n_blocks, MOBA_BLOCK, HEAD_DIM)
    kmean = jnp.mean(kb.astype(jnp.float32), axis=3)
    n_sel = min(MOBA_TOPK, n_blocks)
    scale = HEAD_DIM ** -0.5
    b_idx = jnp.arange(B)[:, None, None, None]
    h_idx = jnp.arange(N_HEADS)[None, :, None, None]
    block_ids = jnp.arange(n_blocks)

    def attend_chunk(c):
        start = c * QUERY_CHUNK
        qc = lax.dynamic_slice_in_dim(q, start, QUERY_CHUNK, axis=2)
        q_pos = start + jnp.arange(QUERY_CHUNK)
        cur = start // MOBA_BLOCK
        gate = jnp.einsum('bhqd,bhnd->bhqn', qc.astype(jnp.float32), kmean)
        gate = jnp.where(block_ids < cur, gate, NEG_INF)
        _, sel = lax.top_k(gate, n_sel)
        sel_valid = sel < cur
        kg = kb[b_idx, h_idx, sel]
        vg = vb[b_idx, h_idx, sel]
        s_past = jnp.einsum('bhqd,bhqknd->bhqkn', qc, kg).astype(jnp.float32) * scale
        s_past = jnp.where(sel_valid[..., None], s_past, NEG_INF)
        k_own = lax.dynamic_index_in_dim(kb, cur, axis=2, keepdims=False)
        v_own = lax.dynamic_index_in_dim(vb, cur, axis=2, keepdims=False)
        key_pos = cur * MOBA_BLOCK + jnp.arange(MOBA_BLOCK)
        s_own = jnp.einsum('bhqd,bhnd->bhqn', qc, k_own).astype(jnp.float32) * scale
        s_own = jnp.where(key_pos[None, :] <= q_pos[:, None], s_own, NEG_INF)
        scores = jnp.concatenate([s_past.reshape(B, N_HEADS, QUERY_CHUNK, n_sel * MOBA_BLOCK), s_own], axis=-1)
        p = jax.nn.softmax(scores, axis=-1).astype(v.dtype)
        p_past = p[..., :n_sel * MOBA_BLOCK].reshape(B, N_HEADS, QUERY_CHUNK, n_sel, MOBA_BLOCK)
        p_own = p[..., n_sel * MOBA_BLOCK:]
        return (jnp.einsum('bhqkn,bhqknd->bhqd', p_past, vg)
                + jnp.einsum('bhqn,bhnd->bhqd', p_own, v_own))

    o = lax.map(attend_chunk, jnp.arange(S // QUERY_CHUNK))
    o = jnp.transpose(o, (1, 0, 3, 2, 4)).reshape(B, S, N_HEADS * HEAD_DIM)
    return o @ w_o


def setup_inputs(seed: int = 0) -> dict:
    key = jax.random.key(seed)
    ks = jax.random.split(key, 20)
    f32 = jnp.float32

    def nrm(k, shape, fan_in):
        return jax.random.normal(k, shape, f32) * (fan_in ** -0.5)

    return {
        'x': jax.random.normal(ks[0], (BATCH, SEQ, D_MODEL), f32),
        'ffn1_w_gate': nrm(ks[1], (DEPTH, D_MODEL, D_FF), D_MODEL),
        'ffn1_w_up': nrm(ks[2], (DEPTH, D_MODEL, D_FF), D_MODEL),
        'ffn1_w_down': nrm(ks[3], (DEPTH, D_FF, D_MODEL), D_FF),
        'ffn2_w_gate': nrm(ks[4], (DEPTH, D_MODEL, D_FF), D_MODEL),
        'ffn2_w_up': nrm(ks[5], (DEPTH, D_MODEL, D_FF), D_MODEL),
        'ffn2_w_down': nrm(ks[6], (DEPTH, D_FF, D_MODEL), D_FF),
        'norm_pre': 1.0 + 0.02 * jax.random.normal(ks[7], (DEPTH, 3, D_MODEL), f32),
        'norm_post': 1.0 + 0.02 * jax.random.normal(ks[8], (DEPTH, 3, D_MODEL), f32),
        'ab_w_in': nrm(ks[9], (N_EVEN, D_MODEL, AB_IN_WIDTH), D_MODEL),
        'pool_w': nrm(ks[10], (N_EVEN, POOL_GROUPS, POOL_GROUP_DIM, POOL_GROUP_DIM), POOL_GROUP_DIM),
        'pool_scale': 1.0 + 0.02 * jax.random.normal(ks[11], (N_EVEN, POOL_GROUPS, POOL_GROUP_DIM), f32),
        'sgu_norm': 1.0 + 0.02 * jax.random.normal(ks[12], (N_EVEN, SGU_GROUPS, SGU_GROUP_DIM), f32),
        'sgu_w': nrm(ks[13], (N_EVEN, SGU_GROUPS, SGU_CHUNK, SGU_CHUNK), SGU_CHUNK),
        'sgu_b': 1.0 + 0.1 * jax.random.normal(ks[14], (N_EVEN, SGU_GROUPS, SGU_CHUNK), f32),
        'ab_w_out': nrm(ks[15], (N_EVEN, AB_OUT_WIDTH, D_MODEL), AB_OUT_WIDTH),
        'attn_w_qkv': nrm(ks[16], (N_ODD, D_MODEL, 3 * N_HEADS * HEAD_DIM), D_MODEL),
        'attn_w_o': nrm(ks[17], (N_ODD, N_HEADS * HEAD_DIM, D_MODEL), N_HEADS * HEAD_DIM),
    }


def reference(x, ffn1_w_gate, ffn1_w_up, ffn1_w_down, ffn2_w_gate, ffn2_w_up, ffn2_w_down,
              norm_pre, norm_post, ab_w_in, pool_w, pool_scale, sgu_norm, sgu_w, sgu_b, ab_w_out,
              attn_w_qkv, attn_w_o):
    h = x
    for layer in range(DEPTH):
        f1 = swiglu(rms_norm(h, norm_pre[layer, 0]), ffn1_w_gate[layer], ffn1_w_up[layer], ffn1_w_down[layer])
        h = h + 0.5 * rms_norm(f1, norm_post[layer, 0])
        m_in = rms_norm(h, norm_pre[layer, 1])
        i = layer // 2
        if layer % 2 == 0:
            m = pool_sgu_mixer(m_in, ab_w_in[i], pool_w[i], pool_scale[i], sgu_norm[i], sgu_w[i], sgu_b[i], ab_w_out[i])
        else:
            m = moba_mixer(m_in, attn_w_qkv[i], attn_w_o[i])
        h = h + rms_norm(m, norm_post[layer, 1])
        f2 = swiglu(rms_norm(h, norm_pre[layer, 2]), ffn2_w_gate[layer], ffn2_w_up[layer], ffn2_w_down[layer])
        h = h + 0.5 * rms_norm(f2, norm_post[layer, 2])
    return h
```

```python
from contextlib import ExitStack

import numpy as np
import concourse.bass as bass
import concourse.mybir as mybir
from concourse.bass_utils import run_bass_kernel_spmd

F32 = mybir.dt.float32
BF16 = mybir.dt.bfloat16
AF = mybir.ActivationFunctionType
ALU = mybir.AluOpType
AX = mybir.AxisListType

P = 128
D = 1024
DFF = 2816
SEQ = 4096
DEPTH = 4
KC = D // P
JC = DFF // P
EPS = 1e-6
NCORES = 8
ARENA_COLS = 52736

ENGS = ("pe", "act", "dve", "pool", "sp")


class Sem:
    def __init__(self, h):
        self.h = h
        self.v = 0


class Buf:
    def __init__(self, ap):
        self.ap = ap
        self.w = None
        self.r = {}


class Sched:
    def __init__(self, nc, es):
        self.nc = nc
        self.es = es
        self.q = {k: [] for k in ENGS}
        self.engsem = {k: self.new_sem("s_" + k) for k in ("pe", "act", "dve", "pool")}
        self.dmasems = []
        self.waited = {}

    def new_sem(self, name):
        self._nsem = getattr(self, "_nsem", 0) + 1
        return Sem(self.es.enter_context(self.nc.semaphore(f"{name}_{self._nsem}")))

    def new_dma_sem(self, name):
        s = self.new_sem(name)
        self.dmasems.append(s)
        return s

    def wait(self, eng, tok):
        if tok is None:
            return
        sem, val = tok
        key = (eng, id(sem))
        if self.waited.get(key, 0) >= val:
            return
        self.waited[key] = val
        h = sem.h
        self.q[eng].append(lambda e: e.wait_ge(h, val))

    def _deps(self, eng, reads, writes, deps):
        for d in deps:
            self.wait(eng, d)
        for b in reads:
            self.wait(eng, b.w)
        for b in writes:
            self.wait(eng, b.w)
            for t in b.r.values():
                self.wait(eng, t)

    def _mark(self, tok, reads, writes):
        for b in reads:
            b.r[id(tok[0])] = tok
        for b in writes:
            b.w = tok
            b.r = {}

    def op(self, eng, fns, reads=(), writes=(), deps=()):
        if not isinstance(fns, (list, tuple)):
            fns = [fns]
        self._deps(eng, reads, writes, deps)
        sem = self.engsem[eng]
        sem.v += 1
        v, h = sem.v, sem.h
        for f in fns[:-1]:
            self.q[eng].append(f)
        last = fns[-1]
        self.q[eng].append(lambda e: last(e).then_inc(h, 1))
        tok = (sem, v)
        self._mark(tok, reads, writes)
        return tok

    def dma(self, eng, fn, sem, reads=(), writes=(), deps=()):
        self._deps(eng, reads, writes, deps)
        sem.v += 16
        v, h = sem.v, sem.h
        self.q[eng].append(lambda e: fn(e).then_inc(h, 16))
        tok = (sem, v)
        self._mark(tok, reads, writes)
        return tok

    def barrier(self):
        toks = [(s, s.v) for s in list(self.engsem.values()) + self.dmasems if s.v > 0]
        for eng in ENGS:
            for t in toks:
                self.wait(eng, t)


class Arena:
    def __init__(self, tensor, ncols):
        self.t = tensor
        self.n = ncols
        self.pos = 0

    def reset(self, pos=0):
        self.pos = pos

    def f32(self, cols):
        a = self.pos
        self.pos += cols
        assert self.pos <= self.n, f"arena overflow {self.pos} > {self.n}"
        return self.t[:, a:a + cols]

    def bf16(self, cols):
        assert cols % 2 == 0
        return self.f32(cols // 2).bitcast(BF16)


class Ctx:
    pass


def rsqrt_chain(S, cx, ss_buf, scale, bias_ap, deps_name=None):
    sd = cx.stat()
    S.op("act", lambda e: e.activation(out=sd.ap, in_=ss_buf.ap, func=AF.Sqrt, bias=bias_ap, scale=scale),
         reads=[ss_buf], writes=[sd])
    rs = cx.stat()
    S.op("dve", lambda e: e.reciprocal(out=rs.ap, in_=sd.ap), reads=[sd], writes=[rs])
    return rs


def make_stats(cx, arena, n=64):
    st = arena.f32(n)
    cx._stats = [Buf(st[:, i:i + 1]) for i in range(n)]
    cx._stat_i = 0

    def stat():
        b = cx._stats[cx._stat_i % n]
        cx._stat_i += 1
        return b
    cx.stat = stat


def load_bcast(S, cx, arena, dram_row_ap, name):
    b = Buf(arena.f32(D))
    sem = S.new_dma_sem("bc_" + name)
    src = dram_row_ap.partition_broadcast(P)
    S.dma("sp", lambda e: e.dma_start(out=b.ap, in_=src), sem, writes=[b])
    return b


def norm_part1(S, cx, arena_bufs, h_src, r0, gpre):
    hin, xs, junk = arena_bufs["hin"], arena_bufs["xs"], arena_bufs["junk"]
    hb = hin[cx.hin_i % len(hin)]
    sem = cx.hin_sems[cx.hin_i % len(hin)]
    cx.hin_i += 1
    S.dma("sp", lambda e: e.dma_start(out=hb.ap, in_=h_src[r0:r0 + P, :]), sem, writes=[hb])
    ss = cx.stat()
    S.op("act", lambda e: e.activation(out=junk.ap, in_=hb.ap, func=AF.Square, accum_out=ss.ap),
         reads=[hb], writes=[junk, ss])
    rs = rsqrt_chain(S, cx, ss, 1.0 / D, cx.eps_ap)
    xb = xs[cx.xs_i % len(xs)]
    cx.xs_i += 1
    S.op("dve", lambda e: e.scalar_tensor_tensor(out=xb.ap, in0=hb.ap, scalar=rs.ap, in1=gpre.ap, op0=ALU.mult, op1=ALU.mult),
         reads=[hb, rs, gpre], writes=[xb])
    return xb


def norm_part2(S, cx, xb, dst, dst_buf, bank_fn=None):
    bank = (bank_fn or cx.next_bank_t)()
    pv = bank.ap.bitcast(BF16).rearrange("p (k t) -> p k t", t=P)
    S.op("pe", [lambda e, k=k: e.transpose(out=pv[:, k, :], in_=xb.ap[:, k * P:(k + 1) * P], identity=cx.ident.ap)
                for k in range(KC)], reads=[xb, cx.ident], writes=[bank])
    S.op("act", lambda e: e.copy(out=dst, in_=pv), reads=[bank], writes=[dst_buf])


def norm_transpose(S, cx, arena_bufs, h_src, row0, nsub, gpre, xnT_bufs, xnT_ap, T, s0=0, bank_fn=None):
    for s in range(s0, s0 + nsub):
        xb = norm_part1(S, cx, arena_bufs, h_src, row0 + s * P, gpre)
        norm_part2(S, cx, xb, xnT_ap[:, :, s * P:(s + 1) * P], xnT_bufs[s], bank_fn)


def post_norm_residual(S, cx, banks2, psum_ap, gpost, h_src, h_dst, r0, half, bufs):
    hep = bufs["hep"][cx.hep_i % len(bufs["hep"])]
    sem = cx.hep_sems[cx.hep_i % len(bufs["hep"])]
    cx.hep_i += 1
    tmp = bufs["tmp"][cx.tmp_i % len(bufs["tmp"])]
    cx.tmp_i += 1
    S.dma("sp", lambda e: e.dma_start(out=hep.ap, in_=h_src[r0:r0 + P, :]), sem, writes=[hep])
    ss = cx.stat()
    S.op("act", lambda e: e.activation(out=tmp.ap, in_=psum_ap, func=AF.Square, accum_out=ss.ap),
         reads=banks2, writes=[tmp, ss])
    if half:
        rs = rsqrt_chain(S, cx, ss, 4.0 / D, cx.eps4_ap)
    else:
        rs = rsqrt_chain(S, cx, ss, 1.0 / D, cx.eps_ap)
    S.op("dve", lambda e: e.tensor_tensor(out=tmp.ap, in0=psum_ap, in1=gpost.ap, op=ALU.mult),
         reads=banks2 + [gpost], writes=[tmp])
    S.op("dve", lambda e: e.scalar_tensor_tensor(out=hep.ap, in0=tmp.ap, scalar=rs.ap, in1=hep.ap,
                                                 op0=ALU.mult, op1=ALU.add),
         reads=[tmp, rs], writes=[hep])
    S.dma("act", lambda e: e.dma_start(out=h_dst[r0:r0 + P, :], in_=hep.ap), sem, reads=[hep])


def setup_common(S, cx, arena, T, norm=True):
    make_stats(cx, arena)
    bufs = {}
    if norm:
        bufs["hin"] = [Buf(arena.f32(D)) for _ in range(2)]
        bufs["xs"] = [Buf(arena.bf16(D)) for _ in range(2)]
        bufs["junk"] = Buf(arena.bf16(D))
    bufs["hep"] = [Buf(arena.f32(D)) for _ in range(3)]
    bufs["tmp"] = [Buf(arena.f32(D)) for _ in range(2)]
    cx.hin_i = cx.xs_i = cx.hep_i = cx.tmp_i = 0
    return bufs


def ffn_stage(S, cx, arena, h_src, h_dst, wg, wu, wd, g_pre, g_post, ntok, T=1024):
    arena.reset(cx.arena_base)
    bufs = setup_common(S, cx, arena, T)
    gpre = load_bcast(S, cx, arena, g_pre, "gpre")
    gpost = load_bcast(S, cx, arena, g_post, "gpost")
    nsub = T // P
    nh = T // 512
    xnT_ap = arena.bf16(KC * T).rearrange("p (k t) -> p k t", t=T)
    xnT_bufs = [Buf(None) for _ in range(nsub)]
    hT_ap = arena.bf16(JC * T).rearrange("p (j t) -> p j t", t=T)
    hT_bufs = [[Buf(None) for _ in range(nh)] for _ in range(JC)]
    wdb_ap = arena.bf16(JC * D).rearrange("p (j d) -> p j d", d=D)
    wdb_bufs = [Buf(None) for _ in range(JC // 2)]
    NST = 4
    stage = [Buf(arena.f32(2048)) for _ in range(NST)]
    wgub = [Buf(arena.bf16(2048)) for _ in range(4)]
    silu = [Buf(arena.f32(512)) for _ in range(2)]
    st_i = 0
    wb_i = 0
    si_i = 0
    wg_v = wg.rearrange("(k p) f -> p k f", p=P)
    wu_v = wu.rearrange("(k p) f -> p k f", p=P)
    wd_v = wd.rearrange("(j p) d -> p j d", p=P)
    NG = JC // 2
    ntile = ntok // T
    wc_tok = {}
    for tile in range(ntile):
        row0 = tile * T
        if tile == 0:
            norm_transpose(S, cx, bufs, h_src, row0, nsub, gpre, xnT_bufs, xnT_ap, T)
        for g in range(NG):
            wtiles = []
            for wi_, wv in enumerate((wg_v, wu_v)):
                wslot = wb_i % 4
                wb = wgub[wslot]
                wb_i += 1
                cache = cx.wc[wi_][g]
                if tile == 0:
                    sb = stage[st_i % NST]
                    ssem = cx.stage_sems[st_i % NST]
                    st_i += 1
                    src = wv[:, :, g * 256:(g + 1) * 256]
                    dst = sb.ap.rearrange("p (k f) -> p k f", f=256)
                    S.dma("sp", lambda e, dst=dst, src=src: e.dma_start(out=dst, in_=src), ssem, writes=[sb])
                    ceng = "pool" if wi_ == 0 else "dve"
                    S.op(ceng, lambda e, wb=wb, sb=sb: e.tensor_copy(out=wb.ap, in_=sb.ap), reads=[sb], writes=[wb])
                    if ntile > 1:
                        wc_tok[(wi_, g)] = S.dma("act", lambda e, wb=wb, cache=cache: e.dma_start(out=cache, in_=wb.ap),
                                                 cx.wgub_sems[wslot], reads=[wb])
                else:
                    S.dma("sp", lambda e, wb=wb, cache=cache: e.dma_start(out=wb.ap, in_=cache), cx.wgub_sems[wslot],
                          writes=[wb], deps=[wc_tok[(wi_, g)]])
                wtiles.append(wb)
            wdst = wdb_ap[:, 2 * g:2 * g + 2, :]
            cache = cx.wc[2][g]
            if tile == 0:
                sb = stage[st_i % NST]
                ssem = cx.stage_sems[st_i % NST]
                st_i += 1
                src = wd_v[:, 2 * g:2 * g + 2, :]
                dst = sb.ap.rearrange("p (j d) -> p j d", d=D)
                S.dma("sp", lambda e, dst=dst, src=src: e.dma_start(out=dst, in_=src), ssem, writes=[sb])
                S.op("act", lambda e, wdst=wdst, dst=dst: e.copy(out=wdst, in_=dst), reads=[sb], writes=[wdb_bufs[g]])
            for c in range(2):
                j = 2 * g + c
                for hf in range(nh):
                    tsl = slice(hf * 512, (hf + 1) * 512)
                    xr = xnT_bufs[hf * 4:(hf + 1) * 4]
                    pbanks = []
                    for wi, wb in enumerate(wtiles):
                        bank = cx.next_bank_a()
                        wv3 = wb.ap.rearrange("p (k f) -> p k f", f=256)
                        S.op("pe", [lambda e, k=k, bank=bank, wv3=wv3, c=c, tsl=tsl: e.matmul(
                            bank.ap, wv3[:, k, c * P:(c + 1) * P], xnT_ap[:, k, tsl], start=(k == 0), stop=(k == KC - 1))
                            for k in range(KC)], reads=[wb] + xr, writes=[bank])
                        pbanks.append(bank)
                    sl = silu[si_i % 2]
                    si_i += 1
                    S.op("act", lambda e, sl=sl, b=pbanks[0]: e.activation(out=sl.ap, in_=b.ap, func=AF.Silu),
                         reads=[pbanks[0]], writes=[sl])
                    hdst = hT_ap[:, j, tsl]
                    S.op("dve", lambda e, hdst=hdst, sl=sl, b=pbanks[1]: e.tensor_tensor(
                        out=hdst, in0=sl.ap, in1=b.ap, op=ALU.mult), reads=[sl, pbanks[1]], writes=[hT_bufs[j][hf]])
        for s in range(nsub):
            b2 = cx.next_bank_pair()
            hf = s // 4
            for dh in range(2):
                S.op("pe", [lambda e, j=j, dh=dh, s=s, bk=b2[dh]: e.matmul(
                    bk.ap, hT_ap[:, j, s * P:(s + 1) * P], wdb_ap[:, j, dh * 512:(dh + 1) * 512],
                    start=(j == 0), stop=(j == JC - 1)) for j in range(JC)],
                    reads=[hT_bufs[j][hf] for j in range(JC)] + wdb_bufs, writes=[b2[dh]])
            post_norm_residual(S, cx, b2, cx.pair_ap(b2), gpost, h_src, h_dst, row0 + s * P, True, bufs)
            if tile + 1 < ntile:
                if s >= 1:
                    norm_part2(S, cx, pend_xb, xnT_ap[:, :, (s - 1) * P:s * P], xnT_bufs[s - 1], cx.next_bank_a)
                pend_xb = norm_part1(S, cx, bufs, h_src, row0 + T + s * P, gpre)
        if tile + 1 < ntile:
            norm_part2(S, cx, pend_xb, xnT_ap[:, :, (nsub - 1) * P:nsub * P], xnT_bufs[nsub - 1], cx.next_bank_a)
    S.barrier()


NEG = -30000.0
POOL_WINDOWS = (2, 4, 8, 16)
MOBA_BLOCK = 256


def make_stager(S, cx, arena, nslots=4):
    st = {"bufs": [Buf(arena.f32(2048)) for _ in range(nslots)], "i": 0}

    def fetch(src3, dst3, dst_bufs, a, b, extra=None):
        k = st["i"] % nslots
        st["i"] += 1
        sb = st["bufs"][k]
        sem = cx.stage_sems[k]
        sv = sb.ap[:, 0:a * b].rearrange("p (a b) -> p a b", b=b)
        S.dma("sp", lambda e: e.dma_start(out=sv, in_=src3), sem, writes=[sb])
        if extra is not None:
            extra(sv, sb)
        S.op("pool", lambda e: e.tensor_copy(out=dst3, in_=sv), reads=[sb], writes=dst_bufs)
    return fetch


def flat_bcast(ap2):
    a, b = ap2.shape
    return ap2.rearrange("(o a) b -> o (a b)", o=1).partition_broadcast(P)


def poolsgu_stage(S, cx, arena, h_src, h_dst, dr, i, layer, ntok, T=512):
    arena.reset(cx.arena_base)
    bufs = setup_common(S, cx, arena, T)
    gpre = load_bcast(S, cx, arena, dr["norm_pre"][layer, 1:2, :], "gpre")
    gpost = load_bcast(S, cx, arena, dr["norm_post"][layer, 1:2, :], "gpost")
    fetch = make_stager(S, cx, arena)
    nsub = T // P
    xnT_ap = arena.bf16(KC * T).rearrange("p (k t) -> p k t", t=T)
    xnT_bufs = [Buf(None) for _ in range(nsub)]
    win_ap = arena.bf16(KC * 1536).rearrange("p (k f) -> p k f", f=1536)
    win_b = Buf(None)
    win_v = dr["ab_w_in"][i].rearrange("(k p) f -> p k f", p=P)
    for g in range(6):
        fetch(win_v[:, :, g * 256:(g + 1) * 256], win_ap[:, :, g * 256:(g + 1) * 256], [win_b], 8, 256)
    wout_ap = arena.bf16(KC * D).rearrange("p (k d) -> p k d", d=D)
    wout_b = Buf(None)
    wout_v = dr["ab_w_out"][i].rearrange("(k p) d -> p k d", p=P)
    for g in range(4):
        fetch(wout_v[:, 2 * g:2 * g + 2, :], wout_ap[:, 2 * g:2 * g + 2, :], [wout_b], 2, 1024)
    pw_ap = arena.bf16(4 * P).rearrange("p (g d) -> p g d", d=P)
    pw_b = Buf(None)
    fetch(dr["pool_w"][i].rearrange("g c d -> c g d"), pw_ap, [pw_b], 4, P)
    sw32 = Buf(arena.f32(4 * P))
    sw3 = sw32.ap.rearrange("p (g s) -> p g s", s=P)
    sem_m = S.new_dma_sem("misc")
    S.dma("sp", lambda e: e.dma_start(out=sw3, in_=dr["sgu_w"][i].rearrange("g t s -> t g s")), sem_m, writes=[sw32])
    swm = Buf(arena.bf16(4 * P))
    swm3 = swm.ap.rearrange("p (g s) -> p g s", s=P)
    tril = cx.cst32.ap[:, 256:384]
    for g in range(4):
        S.op("dve", lambda e, g=g: e.tensor_tensor(out=swm3[:, g, :], in0=sw3[:, g, :], in1=tril, op=ALU.mult),
             reads=[sw32, cx.cst32], writes=[swm])
    wT = Buf(arena.bf16(4 * P))
    wT3 = wT.ap.rearrange("p (g t) -> p g t", t=P)
    bank = cx.next_bank_t()
    pv = bank.ap.bitcast(BF16)[:, 0:4 * P].rearrange("p (g t) -> p g t", t=P)
    S.op("pe", [lambda e, g=g: e.transpose(out=pv[:, g, :], in_=swm3[:, g, :], identity=cx.ident.ap) for g in range(4)],
         reads=[swm, cx.ident], writes=[bank])
    S.op("act", lambda e: e.copy(out=wT3, in_=pv), reads=[bank], writes=[wT])
    psc = Buf(arena.f32(4))
    S.dma("sp", lambda e: e.dma_start(out=psc.ap, in_=dr["pool_scale"][i].rearrange("g d -> d g"),
                                      allow_slow_non_contiguous=True), sem_m, writes=[psc])
    normbc = Buf(arena.f32(512))
    S.dma("sp", lambda e: e.dma_start(out=normbc.ap, in_=flat_bcast(dr["sgu_norm"][i])), sem_m, writes=[normbc])
    bbc = Buf(arena.f32(512))
    S.dma("sp", lambda e: e.dma_start(out=bbc.ap, in_=flat_bcast(dr["sgu_b"][i])), sem_m, writes=[bbc])
    inv0 = Buf(arena.f32(2048))
    S.dma("sp", lambda e: e.dma_start(out=inv0.ap, in_=dr["pcst"][0:1, :].partition_broadcast(P)), sem_m, writes=[inv0])
    inv1 = Buf(arena.f32(2048))
    S.dma("sp", lambda e: e.dma_start(out=inv1.ap, in_=dr["pcst"][1:2, :].partition_broadcast(P)), sem_m, writes=[inv1])
    for _b in (sw32, psc, normbc, bbc, inv0, inv1):
        if _b.w is not None and _b.w[0] is sem_m:
            _b.w = (sem_m, sem_m.v)
    HW = 16
    W = HW + T
    abuf = [Buf(arena.f32(W)) for _ in range(4)]
    sbuf = [Buf(arena.f32(W)) for _ in range(4)]
    for g in range(4):
        S.op("dve", lambda e, g=g: e.memset(abuf[g].ap[:, 0:HW], 0.0), writes=[abuf[g]])
    dT = Buf(arena.bf16(4 * T))
    dT3 = dT.ap.rearrange("p (g t) -> p g t", t=T)
    uT = Buf(arena.f32(4 * T))
    uT3 = uT.ap.rearrange("p (g t) -> p g t", t=T)
    gv = [Buf(arena.f32(512)) for _ in range(2)]
    vn = [Buf(arena.bf16(512)) for _ in range(2)]
    yT = Buf(arena.bf16(8 * T))
    yT3 = yT.ap.rearrange("p (f t) -> p f t", t=T)
    stmp = [Buf(arena.f32(512)) for _ in range(2)]
    ss4 = [Buf(arena.f32(4)) for _ in range(2)]
    sd4 = [Buf(arena.f32(4)) for _ in range(2)]
    rs4 = [Buf(arena.f32(4)) for _ in range(2)]
    vi = 0
    for tile in range(ntok // T):
        row0 = tile * T
        inv = inv0 if tile == 0 else inv1
        inv3 = inv.ap.rearrange("p (g t) -> p g t", t=512)
        norm_transpose(S, cx, bufs, h_src, row0, nsub, gpre, xnT_bufs, xnT_ap, T)
        for zc in range(8):
            bank = cx.next_bank_a()
            S.op("pe", [lambda e, k=k, bank=bank, zc=zc: e.matmul(bank.ap, win_ap[:, k, zc * P:(zc + 1) * P], xnT_ap[:, k, :],
                                                                start=(k == 0), stop=(k == KC - 1)) for k in range(KC)],
                 reads=[win_b] + xnT_bufs, writes=[bank])
            if zc < 4:
                S.op("act", lambda e, bank=bank, zc=zc: e.copy(out=abuf[zc].ap[:, HW:W], in_=bank.ap),
                     reads=[bank], writes=[abuf[zc]])
            else:
                S.op("act", lambda e, bank=bank, zc=zc: e.activation(out=uT3[:, zc - 4, :], in_=bank.ap, func=AF.Gelu),
                     reads=[bank], writes=[uT])
        for g in range(4):
            src = abuf[g]
            for lv in range(g + 1):
                sh = 1 << lv
                lo = 2 * sh - 1
                dst = sbuf[lv]
                S.op("dve", lambda e, src=src, dst=dst, sh=sh, lo=lo: e.tensor_tensor(
                    out=dst.ap[:, lo:W], in0=src.ap[:, lo:W], in1=src.ap[:, lo - sh:W - sh], op=ALU.add),
                    reads=[src], writes=[dst])
                src = dst
            S.op("dve", lambda e, src=src, g=g, inv3=inv3: e.tensor_tensor(out=src.ap[:, HW:W], in0=src.ap[:, HW:W], in1=inv3[:, g, :], op=ALU.mult),
                 reads=[src, inv], writes=[src])
            S.op("dve", lambda e, src=src, g=g: e.tensor_tensor(out=dT3[:, g, :], in0=src.ap[:, HW:W], in1=abuf[g].ap[:, HW:W], op=ALU.subtract),
                 reads=[src, abuf[g]], writes=[dT])
            S.op("dve", lambda e, g=g: e.tensor_copy(out=abuf[g].ap[:, 0:HW], in_=abuf[g].ap[:, T:W]),
                 reads=[abuf[g]], writes=[abuf[g]])
        for g in range(4):
            bank = cx.next_bank_a()
            S.op("pe", lambda e, bank=bank, g=g: e.matmul(bank.ap, pw_ap[:, g, :], dT3[:, g, :], start=True, stop=True),
                 reads=[pw_b, dT], writes=[bank])
            S.op("act", lambda e, bank=bank, g=g: e.activation(out=yT3[:, g, :], in_=bank.ap, func=AF.Copy, scale=psc.ap[:, g:g + 1]),
                 reads=[bank, psc], writes=[yT])
        for ch in range(nsub):
            csl = slice(ch * P, (ch + 1) * P)
            bank = cx.next_bank_a()
            S.op("pe", [lambda e, k=k, bank=bank, csl=csl: e.matmul(bank.ap, xnT_ap[:, k, csl], win_ap[:, k, 1024:1536],
                                                                  start=(k == 0), stop=(k == KC - 1)) for k in range(KC)],
                 reads=[win_b, xnT_bufs[ch]], writes=[bank])
            gb, vb, s4, d4, r4 = gv[vi % 2], vn[vi % 2], ss4[vi % 2], sd4[vi % 2], rs4[vi % 2]
            tb = stmp[vi % 2]
            vi += 1
            S.op("act", lambda e, bank=bank, gb=gb: e.activation(out=gb.ap, in_=bank.ap, func=AF.Gelu), reads=[bank], writes=[gb])
            for g in range(4):
                S.op("act", lambda e, g=g, gb=gb, s4=s4, tb=tb: e.activation(out=tb.ap[:, g * P:(g + 1) * P], in_=gb.ap[:, g * P:(g + 1) * P],
                                                                          func=AF.Square, accum_out=s4.ap[:, g:g + 1]),
                     reads=[gb], writes=[tb, s4])
            S.op("act", lambda e, s4=s4, d4=d4: e.activation(out=d4.ap, in_=s4.ap, func=AF.Sqrt, bias=cx.eps_ap, scale=1.0 / P),
                 reads=[s4], writes=[d4])
            S.op("dve", lambda e, d4=d4, r4=r4: e.reciprocal(out=r4.ap, in_=d4.ap), reads=[d4], writes=[r4])
            for g in range(4):
                S.op("dve", lambda e, g=g, gb=gb, vb=vb, r4=r4: e.scalar_tensor_tensor(
                    out=vb.ap[:, g * P:(g + 1) * P], in0=gb.ap[:, g * P:(g + 1) * P], scalar=r4.ap[:, g:g + 1],
                    in1=normbc.ap[:, g * P:(g + 1) * P], op0=ALU.mult, op1=ALU.mult), reads=[gb, r4, normbc], writes=[vb])
            bank2 = cx.next_bank_a()
            S.op("pe", [lambda e, g=g, bank2=bank2, vb=vb: e.matmul(bank2.ap[:, g * P:(g + 1) * P], vb.ap[:, g * P:(g + 1) * P], wT3[:, g, :],
                                                                  start=True, stop=True) for g in range(4)],
                 reads=[vb, wT], writes=[bank2])
            S.op("dve", lambda e, bank2=bank2, tb=tb: e.tensor_tensor(out=tb.ap, in0=bank2.ap, in1=bbc.ap, op=ALU.add),
                 reads=[bank2, bbc], writes=[tb])
            S.op("dve", lambda e, tb=tb, csl=csl: e.tensor_tensor(out=yT3[:, 4:8, csl], in0=tb.ap.rearrange("p (g t) -> p g t", t=P),
                                                                in1=uT3[:, :, csl], op=ALU.mult), reads=[tb, uT], writes=[yT])
        for ch in range(nsub):
            b2 = cx.next_bank_pair()
            for dh in range(2):
                S.op("pe", [lambda e, f=f, dh=dh, ch=ch, bk=b2[dh]: e.matmul(bk.ap, yT3[:, f, ch * P:(ch + 1) * P], wout_ap[:, f, dh * 512:(dh + 1) * 512],
                                                                          start=(f == 0), stop=(f == 7)) for f in range(8)],
                     reads=[yT, wout_b], writes=[b2[dh]])
            post_norm_residual(S, cx, b2, cx.pair_ap(b2), gpost, h_src, h_dst, row0 + ch * P, False, bufs)
    S.barrier()


def moba_stage(S, cx, arena, h_src, h_dst, dr, i, layer, ntok, T=512):
    NB = ntok // MOBA_BLOCK
    NH = 8
    scale = float(P) ** -0.5
    qT_d, kT_d, v_d = cx.qT_d, cx.kT_d, cx.v_d
    arena.reset(cx.arena_base)
    wo_ap = arena.bf16(KC * D).rearrange("p (k d) -> p k d", d=D)
    wo_b = Buf(None)
    base2 = arena.pos
    bufs = setup_common(S, cx, arena, T)
    gpre = load_bcast(S, cx, arena, dr["norm_pre"][layer, 1:2, :], "gpre")
    fetch = make_stager(S, cx, arena)
    wo_v = dr["attn_w_o"][i].rearrange("(k p) d -> p k d", p=P)
    for g in range(4):
        fetch(wo_v[:, 2 * g:2 * g + 2, :], wo_ap[:, 2 * g:2 * g + 2, :], [wo_b], 2, 1024)
    nsub = T // P
    xnT_ap = arena.bf16(KC * T).rearrange("p (k t) -> p k t", t=T)
    xnT_bufs = [Buf(None) for _ in range(nsub)]
    wq = [Buf(arena.bf16(KC * 256)) for _ in range(3)]
    wr = [Buf(arena.bf16(KC * 256)) for _ in range(3)]
    for _w in wr:
        S.op("pool", lambda e, _w=_w: e.memset(_w.ap, 0.0), writes=[_w])
    rope = [Buf(arena.f32(2 * T)) for _ in range(2)]
    rope_sems = [S.new_dma_sem(f"rope{k}") for k in range(2)]
    t1 = [Buf(arena.f32(T)) for _ in range(2)]
    t2 = [Buf(arena.f32(T)) for _ in range(2)]
    ot = [Buf(arena.bf16(T)) for _ in range(3)]
    ot_sems = [S.new_dma_sem(f"ot{k}") for k in range(3)]
    vt = [Buf(arena.bf16(256)) for _ in range(3)]
    vt_sems = [S.new_dma_sem(f"vt{k}") for k in range(3)]
    wv_all = dr["attn_w_qkv"][i].rearrange("(k p) f -> p k f", p=P)
    wi = ti = oi = vi = 0
    ntile_a = ntok // T
    tokq, tokr = {}, {}
    wq_sems = [S.new_dma_sem(f"wq{k}") for k in range(3)]
    wr_sems = [S.new_dma_sem(f"wr{k}") for k in range(3)]
    for tile in range(ntile_a):
        row0 = tile * T
        norm_transpose(S, cx, bufs, h_src, row0, nsub, gpre, xnT_bufs, xnT_ap, T)
        rb = rope[tile % 2]
        rb3 = rb.ap[0:32, :].rearrange("p (c t) -> p c t", t=T)
        S.dma("sp", lambda e, rb3=rb3, row0=row0: e.dma_start(out=rb3, in_=dr["rope"][:, :, row0:row0 + T]), rope_sems[tile % 2], writes=[rb])
        for g in range(12):
            wb, wrb = wq[wi % 3], wr[wi % 3]
            wi += 1
            wb3 = wb.ap.rearrange("p (k f) -> p k f", f=256)
            wr4 = wrb.ap.rearrange("p (k h c) -> p k h c", h=2, c=P)

            def extra(sv, sb, wrb=wrb, wr4=wr4, g=g):
                if g >= 8:
                    return
                sv4 = sv.rearrange("p k (h c) -> p k h c", c=P)
                S.op("pool", lambda e: e.tensor_copy(out=wr4[:, :, :, 0:16], in_=sv4[:, :, :, 16:32]), reads=[sb], writes=[wrb])
                S.op("pool", lambda e: e.tensor_copy(out=wr4[:, :, :, 16:32], in_=sv4[:, :, :, 0:16]), reads=[sb], writes=[wrb])
            slot = (wi - 1) % 3
            if tile == 0:
                fetch(wv_all[:, :, g * 256:(g + 1) * 256], wb3, [wb], 8, 256, extra=extra)
                if ntile_a > 1:
                    tokq[g] = S.dma("act", lambda e, wb=wb, g=g: e.dma_start(out=cx.wcq[g], in_=wb.ap), wq_sems[slot], reads=[wb])
                    if g < 8:
                        tokr[g] = S.dma("act", lambda e, wrb=wrb, g=g: e.dma_start(out=cx.wcr[g], in_=wrb.ap), wr_sems[slot], reads=[wrb])
            else:
                S.dma("sp", lambda e, wb=wb, g=g: e.dma_start(out=wb.ap, in_=cx.wcq[g]), wq_sems[slot], writes=[wb], deps=[tokq[g]])
                if g < 8:
                    S.dma("sp", lambda e, wrb=wrb, g=g: e.dma_start(out=wrb.ap, in_=cx.wcr[g]), wr_sems[slot], writes=[wrb], deps=[tokr[g]])
            if g < 8:
                dst_d = qT_d if g < 4 else kT_d
                for hh in range(2):
                    h = (g % 4) * 2 + hh
                    bA = cx.next_bank_a()
                    S.op("pe", [lambda e, k=k, bA=bA, hh=hh, wb3=wb3: e.matmul(bA.ap, wb3[:, k, hh * P:(hh + 1) * P], xnT_ap[:, k, :],
                                                                              start=(k == 0), stop=(k == KC - 1)) for k in range(KC)],
                         reads=[wb] + xnT_bufs, writes=[bA])
                    bB = cx.next_bank_a()
                    S.op("pe", [lambda e, k=k, bB=bB, hh=hh, wr4=wr4: e.matmul(bB.ap, wr4[:, k, hh, :], xnT_ap[:, k, :],
                                                                              start=(k == 0), stop=(k == KC - 1)) for k in range(KC)],
                         reads=[wrb] + xnT_bufs, writes=[bB])
                    a1, a2 = t1[ti % 2], t2[ti % 2]
                    ti += 1
                    ob = ot[oi % 3]
                    osem = ot_sems[oi % 3]
                    oi += 1
                    S.op("dve", lambda e, a1=a1, bA=bA, rb3=rb3: e.tensor_tensor(out=a1.ap[0:32, :], in0=bA.ap[0:32, :], in1=rb3[:, 0, :], op=ALU.mult),
                         reads=[bA, rb], writes=[a1])
                    S.op("dve", lambda e, a2=a2, bB=bB, rb3=rb3: e.tensor_tensor(out=a2.ap[0:32, :], in0=bB.ap[0:32, :], in1=rb3[:, 1, :], op=ALU.mult),
                         reads=[bB, rb], writes=[a2])
                    S.op("dve", lambda e, a1=a1, a2=a2, ob=ob: e.tensor_tensor(out=ob.ap[0:32, :], in0=a1.ap[0:32, :], in1=a2.ap[0:32, :], op=ALU.add),
                         reads=[a1, a2], writes=[ob])
                    S.op("act", lambda e, ob=ob, bA=bA: e.copy(out=ob.ap[32:64, :], in_=bA.ap[32:64, :]), reads=[bA], writes=[ob])
                    S.op("act", lambda e, ob=ob, bA=bA: e.copy(out=ob.ap[64:128, :], in_=bA.ap[64:128, :]), reads=[bA], writes=[ob])
                    S.dma("act", lambda e, ob=ob, h=h, dst_d=dst_d, row0=row0: e.dma_start(out=dst_d[h, :, row0:row0 + T], in_=ob.ap),
                          osem, reads=[ob])
            else:
                for ch in range(nsub):
                    bank = cx.next_bank_a()
                    S.op("pe", [lambda e, k=k, bank=bank, ch=ch, wb3=wb3: e.matmul(bank.ap[:, 0:256], xnT_ap[:, k, ch * P:(ch + 1) * P], wb3[:, k, :],
                                                                                  start=(k == 0), stop=(k == KC - 1)) for k in range(KC)],
                         reads=[wb, xnT_bufs[ch]], writes=[bank])
                    vb = vt[vi % 3]
                    vsem = vt_sems[vi % 3]
                    vi += 1
                    S.op("act", lambda e, vb=vb, bank=bank: e.copy(out=vb.ap, in_=bank.ap[:, 0:256]), reads=[bank], writes=[vb])
                    r0 = row0 + ch * P
                    c0 = (g - 8) * 256
                    S.dma("act", lambda e, vb=vb, r0=r0, c0=c0: e.dma_start(out=v_d[r0:r0 + P, c0:c0 + 256], in_=vb.ap), vsem, reads=[vb])
    S.barrier()
    arena.reset(base2)
    bufs = setup_common(S, cx, arena, T, norm=False)
    gpost = load_bcast(S, cx, arena, dr["norm_post"][layer, 1:2, :], "gpost")
    oT_ap = arena.bf16(NH * ntok).rearrange("p (h t) -> p h t", t=ntok)
    oT_b = [Buf(None) for _ in range(NH)]
    kTs = [Buf(arena.bf16(ntok)) for _ in range(2)]
    qTs = [Buf(arena.bf16(ntok)) for _ in range(2)]
    vhs = [Buf(arena.bf16(ntok)) for _ in range(2)]
    hd_sems = [[S.new_dma_sem(f"hd{a}{k}") for k in range(2)] for a in range(3)]
    NQ = ntok // P - 8
    MbT = [Buf(arena.bf16(ntok)) for _ in range(2)]
    for _m in MbT:
        S.op("dve", lambda e, _m=_m: e.memset(_m.ap, 0.0), writes=[_m])
    km32 = Buf(arena.f32(NB))
    kmb = [Buf(arena.bf16(16)) for _ in range(2)]
    Gall = Buf(arena.f32(max(NQ, 1) * 16))
    m8all = Buf(arena.f32(max(NQ, 1) * 8))
    Mball = Buf(arena.bf16(max(NQ, 1) * 16))
    cmask = Buf(arena.f32(max(NQ, 1) * 16))
    PT = [Buf(arena.bf16(512)) for _ in range(3)]
    rec = [Buf(arena.f32(256)) for _ in range(2)]
    E_bf = Buf(arena.bf16(2048))
    e32 = Buf(arena.f32(2048))
    sem_m = S.new_dma_sem("misc2")
    S.dma("sp", lambda e: e.dma_start(out=e32.ap, in_=dr["cst2"]), sem_m, writes=[e32])
    S.op("dve", lambda e: e.tensor_copy(out=E_bf.ap, in_=e32.ap), reads=[e32], writes=[E_bf])
    sem_m3 = S.new_dma_sem("misc3")
    S.dma("sp", lambda e: e.dma_start(out=cmask.ap, in_=dr["cst3"]), sem_m3, writes=[cmask])
    caus = Buf(arena.bf16(512))
    S.op("dve", lambda e: e.tensor_copy(out=caus.ap, in_=cx.cst32.ap[:, 384:896]), reads=[cx.cst32], writes=[caus])
    ones = Buf(arena.bf16(P))
    S.op("dve", lambda e: e.memset(ones.ap, 1.0), writes=[ones])
    bS = [cx.banks[0], cx.banks[1], cx.banks[2]]
    bG = cx.banks[3]
    bO = [cx.banks[4], cx.banks[5]]
    bL = [cx.banks[6], cx.banks[7]]
    si = pi = gi = 0
    v_view = v_d.rearrange("(t p) f -> p t f", p=P)
    for h in range(NH):
        kb, qb, vb = kTs[h % 2], qTs[h % 2], vhs[h % 2]
        S.dma("sp", lambda e, kb=kb, h=h: e.dma_start(out=kb.ap, in_=kT_d[h]), hd_sems[0][h % 2], writes=[kb])
        S.dma("sp", lambda e, qb=qb, h=h: e.dma_start(out=qb.ap, in_=qT_d[h]), hd_sems[1][h % 2], writes=[qb])
        vb3 = vb.ap.rearrange("p (t d) -> p t d", d=P)
        S.dma("sp", lambda e, vb3=vb3, h=h: e.dma_start(out=vb3, in_=v_view[:, :, h * P:(h + 1) * P]), hd_sems[2][h % 2], writes=[vb])
        mbt = MbT[h % 2]
        kmh = kmb[h % 2]
        S.op("dve", lambda e, kb=kb: e.tensor_reduce(out=km32.ap, in_=kb.ap.rearrange("p (n k) -> p n k", k=MOBA_BLOCK), axis=AX.X, op=ALU.add),
             reads=[kb], writes=[km32])
        S.op("dve", lambda e, kmh=kmh: e.tensor_scalar(out=kmh.ap[:, 0:NB], in0=km32.ap, scalar1=1.0 / MOBA_BLOCK, scalar2=None, op0=ALU.mult),
             reads=[km32], writes=[kmh])
        if NQ > 0:
            S.op("pe", [lambda e, qb=qb, j=j, kmh=kmh: e.matmul(bG.ap[:, j * 16:j * 16 + NB], qb.ap[:, (8 + j) * P:(9 + j) * P], kmh.ap[:, 0:NB],
                                                          start=True, stop=True) for j in range(NQ)],
                 reads=[qb, kmh], writes=[bG])
            G3 = Gall.ap.rearrange("p (j n) -> p j n", n=16)
            m83 = m8all.ap.rearrange("p (j n) -> p j n", n=8)
            Mb3 = Mball.ap.rearrange("p (j n) -> p j n", n=16)
            if NB == 16:
                S.op("dve", lambda e: e.tensor_tensor(out=Gall.ap, in0=bG.ap[:, 0:NQ * 16], in1=cmask.ap, op=ALU.add),
                     reads=[bG, cmask], writes=[Gall])
            else:
                S.op("dve", lambda e: e.tensor_copy(out=Gall.ap, in_=cmask.ap), reads=[cmask], writes=[Gall])
                S.op("dve", lambda e, G3=G3: e.tensor_tensor(out=G3[:, :, 0:NB], in0=bG.ap[:, 0:NQ * 16].rearrange("p (j n) -> p j n", n=16)[:, :, 0:NB],
                                                          in1=cmask.ap.rearrange("p (j n) -> p j n", n=16)[:, :, 0:NB], op=ALU.add),
                     reads=[bG, cmask], writes=[Gall])
            S.op("dve", [lambda e, j=j, G3=G3, m83=m83: e.max(out=m83[:, j, :], in_=G3[:, j, :]) for j in range(NQ)],
                 reads=[Gall], writes=[m8all])
            S.op("dve", lambda e, G3=G3, m83=m83, Mb3=Mb3: e.tensor_tensor(out=Mb3, in0=G3, in1=m83[:, :, 2:3].broadcast_to([P, NQ, 16]), op=ALU.is_lt),
                 reads=[Gall, m8all], writes=[Mball])
        def emit_pv(c, n, pt, kb=kb, qb=qb, vb=vb, vb3=vb3, h=h):
            O, L = bO[c % 2], bL[c % 2]
            qsl = slice(c * 256, (c + 1) * 256)
            fns = []
            for kk in range(2):
                kt = 2 * n + kk
                st_ = (n == 0 and kk == 0)
                sp_ = (n == c and kk == 1)
                fns.append(lambda e, pt=pt, kk=kk, kt=kt, st_=st_, sp_=sp_, O=O, vb3=vb3: e.matmul(
                    O.ap[:, 0:256], vb3[:, kt, :], pt.ap[:, kk * 256:(kk + 1) * 256], start=st_, stop=sp_))
                fns.append(lambda e, pt=pt, kk=kk, st_=st_, sp_=sp_, L=L: e.matmul(
                    L.ap[:, 0:256], ones.ap, pt.ap[:, kk * 256:(kk + 1) * 256], start=st_, stop=sp_))
            if n == 0:
                S.op("pe", fns, reads=[vb, pt, ones], writes=[O, L])
            else:
                S.op("pe", fns, reads=[vb, pt, ones, O, L])
                tok = (S.engsem["pe"], S.engsem["pe"].v)
                O.w = tok
                L.w = tok
            if n == c:
                rc = rec[c % 2]
                S.op("dve", lambda e, rc=rc, L=L: e.reciprocal(out=rc.ap, in_=L.ap[:, 0:256]), reads=[L], writes=[rc])
                S.op("dve", lambda e, rc=rc, O=O, h=h, qsl=qsl: e.tensor_tensor(out=oT_ap[:, h, qsl], in0=O.ap[:, 0:256], in1=rc.ap, op=ALU.mult),
                     reads=[O, rc], writes=[oT_b[h]])

        pend = None
        for c in range(NB):
            qsl = slice(c * 256, (c + 1) * 256)
            if c == 4 and NQ > 0:
                pvb = bG.ap.bitcast(BF16)
                for g0 in range(0, NQ, 8):
                    ng = min(8, NQ - g0)
                    S.op("pe", [lambda e, j=j, g0=g0, Mb3=Mb3, pvb=pvb: e.transpose(out=pvb[0:16, (j - g0) * P:(j - g0 + 1) * P], in_=Mb3[:, j, :],
                                                                                  identity=cx.ident.ap) for j in range(g0, g0 + ng)],
                         reads=[Mball, cx.ident], writes=[bG])
                    S.op("act", lambda e, mbt=mbt, g0=g0, ng=ng, pvb=pvb: e.copy(out=mbt.ap[0:16, (8 + g0) * P:(8 + g0 + ng) * P], in_=pvb[0:16, 0:ng * P]),
                         reads=[bG], writes=[mbt])
            for n in range(c + 1):
                Sb = bS[si % 3]
                si += 1
                fns = []
                for kk in range(2):
                    kt = 2 * n + kk
                    osl = slice(kk * 256, (kk + 1) * 256)
                    masked = (n == c) or (c >= 4)
                    fns.append(lambda e, Sb=Sb, osl=osl, kt=kt, masked=masked, kb=kb, qb=qb, qsl=qsl: e.matmul(
                        Sb.ap[:, osl], kb.ap[:, kt * P:(kt + 1) * P], qb.ap[:, qsl], start=True, stop=not masked))
                    if n == c:
                        fns.append(lambda e, Sb=Sb, osl=osl, kk=kk: e.matmul(
                            Sb.ap[:, osl], cx.ident.ap, caus.ap[:, kk * 256:(kk + 1) * 256], start=False, stop=True))
                    elif c >= 4:
                        fns.append(lambda e, Sb=Sb, osl=osl, n=n, mbt=mbt, qsl=qsl: e.matmul(
                            Sb.ap[:, osl], E_bf.ap[:, n * P:(n + 1) * P], mbt.ap[:, qsl], start=False, stop=True))
                S.op("pe", fns, reads=[kb, qb, cx.ident, caus, E_bf, mbt], writes=[Sb])
                pt = PT[pi % 3]
                pi += 1
                S.op("act", lambda e, pt=pt, Sb=Sb: e.activation(out=pt.ap, in_=Sb.ap, func=AF.Exp, scale=scale), reads=[Sb], writes=[pt])
                if pend is not None:
                    emit_pv(*pend)
                pend = (c, n, pt)
        emit_pv(*pend)
    for ch in range(ntok // P):
        b2 = [cx.banks[0], cx.banks[1]] if ch % 2 == 0 else [cx.banks[2], cx.banks[3]]
        for dh in range(2):
            S.op("pe", [lambda e, hh=hh, dh=dh, ch=ch, bk=b2[dh]: e.matmul(bk.ap, oT_ap[:, hh, ch * P:(ch + 1) * P], wo_ap[:, hh, dh * 512:(dh + 1) * 512],
                                                                        start=(hh == 0), stop=(hh == NH - 1)) for hh in range(NH)],
                 reads=oT_b + [wo_b], writes=[b2[dh]])
        post_norm_residual(S, cx, b2, cx.pair_ap(b2), gpost, h_src, h_dst, ch * P, False, bufs)
    S.barrier()


def stage_inputs(stages):
    need = {"x", "norm_pre", "norm_post", "cst"}
    for st in stages:
        if st[0] == "ffn":
            pre = "ffn1" if st[2] == 0 else "ffn2"
            need |= {pre + "_w_gate", pre + "_w_up", pre + "_w_down"}
        elif st[0] == "poolsgu":
            need |= {"ab_w_in", "pool_w", "pool_scale", "sgu_norm", "sgu_w", "sgu_b", "ab_w_out", "pcst"}
        elif st[0] == "moba":
            need |= {"attn_w_qkv", "attn_w_o", "rope", "cst2", "cst3"}
    return need


def input_shapes(ntok):
    sh = {"x": (ntok, D), "norm_pre": (DEPTH, 3, D), "norm_post": (DEPTH, 3, D), "cst": (P, 1024),
          "ab_w_in": (2, D, 1536), "pool_w": (2, 4, P, P), "pool_scale": (2, 4, P), "sgu_norm": (2, 4, P),
          "sgu_w": (2, 4, P, P), "sgu_b": (2, 4, P), "ab_w_out": (2, D, D), "pcst": (2, 2048),
          "attn_w_qkv": (2, D, 3 * D), "attn_w_o": (2, D, D), "rope": (32, 2, ntok), "cst2": (P, 2048),
          "cst3": (P, max(ntok // P - 8, 1) * 16)}
    for f in ("ffn1", "ffn2"):
        sh[f + "_w_gate"] = (DEPTH, D, DFF)
        sh[f + "_w_up"] = (DEPTH, D, DFF)
        sh[f + "_w_down"] = (DEPTH, DFF, D)
    return sh


def build_program(stages, ntok=SEQ):
    nc = bass.Bass("TRN2", target_bir_lowering=False)
    dr = {}
    shapes = input_shapes(ntok)
    for name in sorted(stage_inputs(stages)):
        dr[name] = nc.dram_tensor(name, list(shapes[name]), F32, kind="ExternalInput").ap()
    out = nc.dram_tensor("out", [ntok, D], F32, kind="ExternalOutput").ap()

    es = ExitStack()
    with es:
        arena_t = es.enter_context(nc.sbuf_tensor("arena", [P, ARENA_COLS], F32))
        psum_t = es.enter_context(nc.psum_tensor("psum", [P, 4096], F32))
        S = Sched(nc, es)
        cx = Ctx()
        if any(st[0] == "moba" for st in stages):
            cx.qT_d = nc.dram_tensor("qT_d", [8, P, ntok], BF16).ap()
            cx.kT_d = nc.dram_tensor("kT_d", [8, P, ntok], BF16).ap()
            cx.v_d = nc.dram_tensor("v_d", [ntok, D], BF16).ap()
            cx.wcq = nc.dram_tensor("wcq", [12, P, 2048], BF16).ap()
            cx.wcr = nc.dram_tensor("wcr", [8, P, 2048], BF16).ap()
        arena = Arena(arena_t, ARENA_COLS)
        cst32 = Buf(arena.f32(1024))
        csem = S.new_dma_sem("cst")
        S.dma("sp", lambda e: e.dma_start(out=cst32.ap, in_=dr["cst"]), csem, writes=[cst32])
        cx.ident = Buf(arena.bf16(P))
        S.op("dve", lambda e: e.tensor_copy(out=cx.ident.ap, in_=cst32.ap[:, 0:P]), reads=[cst32], writes=[cx.ident])
        cx.eps_ap = cst32.ap[:, 128:129]
        cx.eps4_ap = cst32.ap[:, 129:130]
        cx.cst32 = cst32
        cx.arena_base = arena.pos
        cx.banks = [Buf(psum_t[:, i * 512:(i + 1) * 512]) for i in range(8)]
        cx.ba_i = cx.bt_i = cx.bp_i = 0

        def next_bank_a():
            b = cx.banks[cx.ba_i % 4]
            cx.ba_i += 1
            return b

        def next_bank_t():
            b = cx.banks[4 + cx.bt_i % 4]
            cx.bt_i += 1
            return b

        def next_bank_pair():
            k = 4 + 2 * (cx.bp_i % 2)
            cx.bp_i += 1
            return [cx.banks[k], cx.banks[k + 1]]

        def pair_ap(b2):
            k = cx.banks.index(b2[0])
            return psum_t[:, k * 512:(k + 2) * 512]
        cx.next_bank_a, cx.next_bank_t, cx.next_bank_pair, cx.pair_ap = next_bank_a, next_bank_t, next_bank_pair, pair_ap
        cx.hin_sems = [S.new_dma_sem(f"hin{k}") for k in range(2)]
        cx.hep_sems = [S.new_dma_sem(f"hep{k}") for k in range(3)]
        cx.stage_sems = [S.new_dma_sem(f"stg{k}") for k in range(4)]
        cx.wgub_sems = [S.new_dma_sem(f"wgub{k}") for k in range(4)]
        cx.wdb_sems = [S.new_dma_sem(f"wdb{k}") for k in range(JC // 2)]
        cx.wc = [nc.dram_tensor(f"wc_{n_}", [JC // 2, P, 2048], BF16).ap() for n_ in ("g", "u", "d")]

        first = True
        for st in stages:
            h_src = dr["x"] if first else out
            first = False
            if st[0] == "ffn":
                _, layer, which = st
                pre = "ffn1" if which == 0 else "ffn2"
                nk = 0 if which == 0 else 2
                ffn_stage(S, cx, arena, h_src, out,
                          dr[pre + "_w_gate"][layer], dr[pre + "_w_up"][layer], dr[pre + "_w_down"][layer],
                          dr["norm_pre"][layer, nk:nk + 1, :], dr["norm_post"][layer, nk:nk + 1, :], ntok)
            elif st[0] == "poolsgu":
                poolsgu_stage(S, cx, arena, h_src, out, dr, st[1] // 2, st[1], ntok)
            elif st[0] == "moba":
                moba_stage(S, cx, arena, h_src, out, dr, st[1] // 2, st[1], ntok)
            else:
                raise ValueError(st)

        with nc.Block() as block:
            @block.tensor
            def _(e):
                for f in S.q["pe"]:
                    f(e)

            @block.scalar
            def _(e):
                for f in S.q["act"]:
                    f(e)

            @block.vector
            def _(e):
                for f in S.q["dve"]:
                    f(e)

            @block.gpsimd
            def _(e):
                for f in S.q["pool"]:
                    f(e)

            @block.sync
            def _(e):
                for f in S.q["sp"]:
                    f(e)
    return nc


def make_consts(ntok=SEQ):
    c = np.zeros((P, 1024), np.float32)
    c[:, 0:P] = np.eye(P, dtype=np.float32)
    c[:, 128] = EPS
    c[:, 129] = 4 * EPS
    t = np.arange(P)
    c[:, 256:384] = (t[None, :] <= t[:, None]).astype(np.float32)
    q = np.arange(256)
    for kk in range(2):
        c[:, 384 + kk * 256:384 + (kk + 1) * 256] = np.where(q[None, :] >= t[:, None] + 128 * kk, 0.0, NEG)
    c2 = np.zeros((P, 2048), np.float32)
    for n in range(16):
        c2[n, n * P:(n + 1) * P] = NEG
    nq = max(ntok // P - 8, 1)
    c3 = np.zeros((P, nq, 16), np.float32)
    for j in range(nq):
        c3[:, j, (8 + j) // 2:] = -1e30
    pc = np.zeros((2, 4, 512), np.float32)
    tt = np.arange(512)
    for g, w in enumerate(POOL_WINDOWS):
        pc[0, g] = 1.0 / np.minimum(tt + 1, w)
        pc[1, g] = 1.0 / w
    pos = np.arange(ntok, dtype=np.float32)
    inv_freq = (1.0 / (np.float32(500000.0) ** (np.arange(0, 32, 2, dtype=np.float32) / np.float32(32)))).astype(np.float32)
    ang = pos[None, :] * inv_freq[:, None]
    rope = np.zeros((32, 2, ntok), np.float32)
    rope[0:16, 0] = np.cos(ang)
    rope[16:32, 0] = np.cos(ang)
    rope[0:16, 1] = -np.sin(ang)
    rope[16:32, 1] = np.sin(ang)
    return {"cst": c, "cst2": c2, "cst3": c3.reshape(P, nq * 16), "pcst": pc.reshape(2, 2048), "rope": rope}


ALL_STAGES = []
for _l in range(DEPTH):
    ALL_STAGES += [("ffn", _l, 0), ("poolsgu" if _l % 2 == 0 else "moba", _l), ("ffn", _l, 1)]


def kernel(**inputs):
    stages = ALL_STAGES
    nc = build_program(stages, SEQ)
    need = stage_inputs(stages)
    consts = make_consts(SEQ)
    shared = {}
    for name in need:
        if name in consts:
            shared[name] = consts[name]
        elif name != "x":
            shared[name] = np.ascontiguousarray(np.asarray(inputs[name], dtype=np.float32))
    x = np.asarray(inputs["x"], dtype=np.float32)
    in_maps = []
    for c in range(NCORES):
        m = dict(shared)
        m["x"] = np.ascontiguousarray(x[c])
        in_maps.append(m)
    res = run_bass_kernel_spmd(nc, in_maps, core_ids=list(range(NCORES)))
    return np.stack([np.asarray(r["out"]) for r in res.results], axis=0).astype(np.float32)
```

```python
from contextlib import ExitStack

import numpy as np
import concourse.bass as bass
import concourse.mybir as mybir
from concourse.bass_utils import run_bass_kernel_spmd

F32 = mybir.dt.float32
BF16 = mybir.dt.bfloat16
AF = mybir.ActivationFunctionType
ALU = mybir.AluOpType
AX = mybir.AxisListType

P = 128
D = 1024
DFF = 2816
SEQ = 4096
DEPTH = 4
KC = D // P
JC = DFF // P
EPS = 1e-6
NCORES = 8
ARENA_COLS = 52736

ENGS = ("pe", "act", "dve", "pool", "sp")


class Sem:
    def __init__(self, h):
        self.h = h
        self.v = 0


class Buf:
    def __init__(self, ap):
        self.ap = ap
        self.w = None
        self.r = {}


class Sched:
    def __init__(self, nc, es):
        self.nc = nc
        self.es = es
        self.q = {k: [] for k in ENGS}
        self.engsem = {k: self.new_sem("s_" + k) for k in ("pe", "act", "dve", "pool")}
        self.dmasems = []
        self.waited = {}

    def new_sem(self, name):
        self._nsem = getattr(self, "_nsem", 0) + 1
        return Sem(self.es.enter_context(self.nc.semaphore(f"{name}_{self._nsem}")))

    def new_dma_sem(self, name):
        s = self.new_sem(name)
        self.dmasems.append(s)
        return s

    def wait(self, eng, tok):
        if tok is None:
            return
        sem, val = tok
        key = (eng, id(sem))
        if self.waited.get(key, 0) >= val:
            return
        self.waited[key] = val
        h = sem.h
        self.q[eng].append(lambda e: e.wait_ge(h, val))

    def _deps(self, eng, reads, writes, deps):
        for d in deps:
            self.wait(eng, d)
        for b in reads:
            self.wait(eng, b.w)
        for b in writes:
            self.wait(eng, b.w)
            for t in b.r.values():
                self.wait(eng, t)

    def _mark(self, tok, reads, writes):
        for b in reads:
            b.r[id(tok[0])] = tok
        for b in writes:
            b.w = tok
            b.r = {}

    def op(self, eng, fns, reads=(), writes=(), deps=()):
        if not isinstance(fns, (list, tuple)):
            fns = [fns]
        self._deps(eng, reads, writes, deps)
        sem = self.engsem[eng]
        sem.v += 1
        v, h = sem.v, sem.h
        for f in fns[:-1]:
            self.q[eng].append(f)
        last = fns[-1]
        self.q[eng].append(lambda e: last(e).then_inc(h, 1))
        tok = (sem, v)
        self._mark(tok, reads, writes)
        return tok

    def dma(self, eng, fn, sem, reads=(), writes=(), deps=()):
        self._deps(eng, reads, writes, deps)
        sem.v += 16
        v, h = sem.v, sem.h
        self.q[eng].append(lambda e: fn(e).then_inc(h, 16))
        tok = (sem, v)
        self._mark(tok, reads, writes)
        return tok

    def barrier(self):
        toks = [(s, s.v) for s in list(self.engsem.values()) + self.dmasems if s.v > 0]
        for eng in ENGS:
            for t in toks:
                self.wait(eng, t)


class Arena:
    def __init__(self, tensor, ncols):
        self.t = tensor
        self.n = ncols
        self.pos = 0

    def reset(self, pos=0):
        self.pos = pos

    def f32(self, cols):
        a = self.pos
        self.pos += cols
        assert self.pos <= self.n, f"arena overflow {self.pos} > {self.n}"
        return self.t[:, a:a + cols]

    def bf16(self, cols):
        assert cols % 2 == 0
        return self.f32(cols // 2).bitcast(BF16)


class Ctx:
    pass


def rsqrt_chain(S, cx, ss_buf, scale, bias_ap, deps_name=None):
    sd = cx.stat()
    S.op("act", lambda e: e.activation(out=sd.ap, in_=ss_buf.ap, func=AF.Sqrt, bias=bias_ap, scale=scale),
         reads=[ss_buf], writes=[sd])
    rs = cx.stat()
    S.op("dve", lambda e: e.reciprocal(out=rs.ap, in_=sd.ap), reads=[sd], writes=[rs])
    return rs


def make_stats(cx, arena, n=64):
    st = arena.f32(n)
    cx._stats = [Buf(st[:, i:i + 1]) for i in range(n)]
    cx._stat_i = 0

    def stat():
        b = cx._stats[cx._stat_i % n]
        cx._stat_i += 1
        return b
    cx.stat = stat


def load_bcast(S, cx, arena, dram_row_ap, name):
    b = Buf(arena.f32(D))
    sem = S.new_dma_sem("bc_" + name)
    src = dram_row_ap.partition_broadcast(P)
    S.dma("sp", lambda e: e.dma_start(out=b.ap, in_=src), sem, writes=[b])
    return b


def norm_part1(S, cx, arena_bufs, h_src, r0, gpre):
    hin, xs, junk = arena_bufs["hin"], arena_bufs["xs"], arena_bufs["junk"]
    hb = hin[cx.hin_i % len(hin)]
    sem = cx.hin_sems[cx.hin_i % len(hin)]
    cx.hin_i += 1
    S.dma("sp", lambda e: e.dma_start(out=hb.ap, in_=h_src[r0:r0 + P, :]), sem, writes=[hb])
    ss = cx.stat()
    S.op("act", lambda e: e.activation(out=junk.ap, in_=hb.ap, func=AF.Square, accum_out=ss.ap),
         reads=[hb], writes=[junk, ss])
    rs = rsqrt_chain(S, cx, ss, 1.0 / D, cx.eps_ap)
    xb = xs[cx.xs_i % len(xs)]
    cx.xs_i += 1
    S.op("dve", lambda e: e.scalar_tensor_tensor(out=xb.ap, in0=hb.ap, scalar=rs.ap, in1=gpre.ap, op0=ALU.mult, op1=ALU.mult),
         reads=[hb, rs, gpre], writes=[xb])
    return xb


def norm_part2(S, cx, xb, dst, dst_buf, bank_fn=None):
    bank = (bank_fn or cx.next_bank_t)()
    pv = bank.ap.bitcast(BF16).rearrange("p (k t) -> p k t", t=P)
    S.op("pe", [lambda e, k=k: e.transpose(out=pv[:, k, :], in_=xb.ap[:, k * P:(k + 1) * P], identity=cx.ident.ap)
                for k in range(KC)], reads=[xb, cx.ident], writes=[bank])
    S.op("act", lambda e: e.copy(out=dst, in_=pv), reads=[bank], writes=[dst_buf])


def norm_transpose(S, cx, arena_bufs, h_src, row0, nsub, gpre, xnT_bufs, xnT_ap, T, s0=0, bank_fn=None):
    for s in range(s0, s0 + nsub):
        xb = norm_part1(S, cx, arena_bufs, h_src, row0 + s * P, gpre)
        norm_part2(S, cx, xb, xnT_ap[:, :, s * P:(s + 1) * P], xnT_bufs[s], bank_fn)


def post_norm_residual(S, cx, banks2, psum_ap, gpost, h_src, h_dst, r0, half, bufs):
    hep = bufs["hep"][cx.hep_i % len(bufs["hep"])]
    sem = cx.hep_sems[cx.hep_i % len(bufs["hep"])]
    cx.hep_i += 1
    tmp = bufs["tmp"][cx.tmp_i % len(bufs["tmp"])]
    cx.tmp_i += 1
    S.dma("sp", lambda e: e.dma_start(out=hep.ap, in_=h_src[r0:r0 + P, :]), sem, writes=[hep])
    ss = cx.stat()
    S.op("act", lambda e: e.activation(out=tmp.ap, in_=psum_ap, func=AF.Square, accum_out=ss.ap),
         reads=banks2, writes=[tmp, ss])
    if half:
        rs = rsqrt_chain(S, cx, ss, 4.0 / D, cx.eps4_ap)
    else:
        rs = rsqrt_chain(S, cx, ss, 1.0 / D, cx.eps_ap)
    S.op("dve", lambda e: e.tensor_tensor(out=tmp.ap, in0=psum_ap, in1=gpost.ap, op=ALU.mult),
         reads=banks2 + [gpost], writes=[tmp])
    S.op("dve", lambda e: e.scalar_tensor_tensor(out=hep.ap, in0=tmp.ap, scalar=rs.ap, in1=hep.ap,
                                                 op0=ALU.mult, op1=ALU.add),
         reads=[tmp, rs], writes=[hep])
    S.dma("act", lambda e: e.dma_start(out=h_dst[r0:r0 + P, :], in_=hep.ap), sem, reads=[hep])


def setup_common(S, cx, arena, T, norm=True):
    make_stats(cx, arena)
    bufs = {}
    if norm:
        bufs["hin"] = [Buf(arena.f32(D)) for _ in range(2)]
        bufs["xs"] = [Buf(arena.bf16(D)) for _ in range(2)]
        bufs["junk"] = Buf(arena.bf16(D))
    bufs["hep"] = [Buf(arena.f32(D)) for _ in range(3)]
    bufs["tmp"] = [Buf(arena.f32(D)) for _ in range(2)]
    cx.hin_i = cx.xs_i = cx.hep_i = cx.tmp_i = 0
    return bufs


def ffn_stage(S, cx, arena, h_src, h_dst, wg, wu, wd, g_pre, g_post, ntok, T=1024):
    arena.reset(cx.arena_base)
    bufs = setup_common(S, cx, arena, T)
    gpre = load_bcast(S, cx, arena, g_pre, "gpre")
    gpost = load_bcast(S, cx, arena, g_post, "gpost")
    nsub = T // P
    nh = T // 512
    xnT_ap = arena.bf16(KC * T).rearrange("p (k t) -> p k t", t=T)
    xnT_bufs = [Buf(None) for _ in range(nsub)]
    hT_ap = arena.bf16(JC * T).rearrange("p (j t) -> p j t", t=T)
    hT_bufs = [[Buf(None) for _ in range(nh)] for _ in range(JC)]
    wdb_ap = arena.bf16(JC * D).rearrange("p (j d) -> p j d", d=D)
    wdb_bufs = [Buf(None) for _ in range(JC // 2)]
    NST = 4
    stage = [Buf(arena.f32(2048)) for _ in range(NST)]
    wgub = [Buf(arena.bf16(2048)) for _ in range(4)]
    silu = [Buf(arena.f32(512)) for _ in range(2)]
    st_i = 0
    wb_i = 0
    si_i = 0
    wg_v = wg.rearrange("(k p) f -> p k f", p=P)
    wu_v = wu.rearrange("(k p) f -> p k f", p=P)
    wd_v = wd.rearrange("(j p) d -> p j d", p=P)
    NG = JC // 2
    ntile = ntok // T
    wc_tok = {}
    for tile in range(ntile):
        row0 = tile * T
        if tile == 0:
            norm_transpose(S, cx, bufs, h_src, row0, nsub, gpre, xnT_bufs, xnT_ap, T)
        for g in range(NG):
            wtiles = []
            for wi_, wv in enumerate((wg_v, wu_v)):
                wslot = wb_i % 4
                wb = wgub[wslot]
                wb_i += 1
                cache = cx.wc[wi_][g]
                if tile == 0:
                    sb = stage[st_i % NST]
                    ssem = cx.stage_sems[st_i % NST]
                    st_i += 1
                    src = wv[:, :, g * 256:(g + 1) * 256]
                    dst = sb.ap.rearrange("p (k f) -> p k f", f=256)
                    S.dma("sp", lambda e, dst=dst, src=src: e.dma_start(out=dst, in_=src), ssem, writes=[sb])
                    ceng = "pool" if wi_ == 0 else "dve"
                    S.op(ceng, lambda e, wb=wb, sb=sb: e.tensor_copy(out=wb.ap, in_=sb.ap), reads=[sb], writes=[wb])
                    if ntile > 1:
                        wc_tok[(wi_, g)] = S.dma("act", lambda e, wb=wb, cache=cache: e.dma_start(out=cache, in_=wb.ap),
                                                 cx.wgub_sems[wslot], reads=[wb])
                else:
                    S.dma("sp", lambda e, wb=wb, cache=cache: e.dma_start(out=wb.ap, in_=cache), cx.wgub_sems[wslot],
                          writes=[wb], deps=[wc_tok[(wi_, g)]])
                wtiles.append(wb)
            wdst = wdb_ap[:, 2 * g:2 * g + 2, :]
            cache = cx.wc[2][g]
            if tile == 0:
                sb = stage[st_i % NST]
                ssem = cx.stage_sems[st_i % NST]
                st_i += 1
                src = wd_v[:, 2 * g:2 * g + 2, :]
                dst = sb.ap.rearrange("p (j d) -> p j d", d=D)
                S.dma("sp", lambda e, dst=dst, src=src: e.dma_start(out=dst, in_=src), ssem, writes=[sb])
                S.op("act", lambda e, wdst=wdst, dst=dst: e.copy(out=wdst, in_=dst), reads=[sb], writes=[wdb_bufs[g]])
            for c in range(2):
                j = 2 * g + c
                for hf in range(nh):
                    tsl = slice(hf * 512, (hf + 1) * 512)
                    xr = xnT_bufs[hf * 4:(hf + 1) * 4]
                    pbanks = []
                    for wi, wb in enumerate(wtiles):
                        bank = cx.next_bank_a()
                        wv3 = wb.ap.rearrange("p (k f) -> p k f", f=256)
                        S.op("pe", [lambda e, k=k, bank=bank, wv3=wv3, c=c, tsl=tsl: e.matmul(
                            bank.ap, wv3[:, k, c * P:(c + 1) * P], xnT_ap[:, k, tsl], start=(k == 0), stop=(k == KC - 1))
                            for k in range(KC)], reads=[wb] + xr, writes=[bank])
                        pbanks.append(bank)
                    sl = silu[si_i % 2]
                    si_i += 1
                    S.op("act", lambda e, sl=sl, b=pbanks[0]: e.activation(out=sl.ap, in_=b.ap, func=AF.Silu),
                         reads=[pbanks[0]], writes=[sl])
                    hdst = hT_ap[:, j, tsl]
                    S.op("dve", lambda e, hdst=hdst, sl=sl, b=pbanks[1]: e.tensor_tensor(
                        out=hdst, in0=sl.ap, in1=b.ap, op=ALU.mult), reads=[sl, pbanks[1]], writes=[hT_bufs[j][hf]])
        for s in range(nsub):
            b2 = cx.next_bank_pair()
            hf = s // 4
            for dh in range(2):
                S.op("pe", [lambda e, j=j, dh=dh, s=s, bk=b2[dh]: e.matmul(
                    bk.ap, hT_ap[:, j, s * P:(s + 1) * P], wdb_ap[:, j, dh * 512:(dh + 1) * 512],
                    start=(j == 0), stop=(j == JC - 1)) for j in range(JC)],
                    reads=[hT_bufs[j][hf] for j in range(JC)] + wdb_bufs, writes=[b2[dh]])
            post_norm_residual(S, cx, b2, cx.pair_ap(b2), gpost, h_src, h_dst, row0 + s * P, True, bufs)
            if tile + 1 < ntile:
                if s >= 1:
                    norm_part2(S, cx, pend_xb, xnT_ap[:, :, (s - 1) * P:s * P], xnT_bufs[s - 1], cx.next_bank_a)
                pend_xb = norm_part1(S, cx, bufs, h_src, row0 + T + s * P, gpre)
        if tile + 1 < ntile:
            norm_part2(S, cx, pend_xb, xnT_ap[:, :, (nsub - 1) * P:nsub * P], xnT_bufs[nsub - 1], cx.next_bank_a)
    S.barrier()


NEG = -30000.0
POOL_WINDOWS = (2, 4, 8, 16)
MOBA_BLOCK = 256


def make_stager(S, cx, arena, nslots=4):
    st = {"bufs": [Buf(arena.f32(2048)) for _ in range(nslots)], "i": 0}

    def fetch(src3, dst3, dst_bufs, a, b, extra=None):
        k = st["i"] % nslots
        st["i"] += 1
        sb = st["bufs"][k]
        sem = cx.stage_sems[k]
        sv = sb.ap[:, 0:a * b].rearrange("p (a b) -> p a b", b=b)
        S.dma("sp", lambda e: e.dma_start(out=sv, in_=src3), sem, writes=[sb])
        if extra is not None:
            extra(sv, sb)
        S.op("pool", lambda e: e.tensor_copy(out=dst3, in_=sv), reads=[sb], writes=dst_bufs)
    return fetch


def flat_bcast(ap2):
    a, b = ap2.shape
    return ap2.rearrange("(o a) b -> o (a b)", o=1).partition_broadcast(P)


def poolsgu_stage(S, cx, arena, h_src, h_dst, dr, i, layer, ntok, T=512):
    arena.reset(cx.arena_base)
    bufs = setup_common(S, cx, arena, T)
    gpre = load_bcast(S, cx, arena, dr["norm_pre"][layer, 1:2, :], "gpre")
    gpost = load_bcast(S, cx, arena, dr["norm_post"][layer, 1:2, :], "gpost")
    stg_pos = arena.pos
    fetch = make_stager(S, cx, arena)
    nsub = T // P
    xnT_ap = arena.bf16(KC * T).rearrange("p (k t) -> p k t", t=T)
    xnT_bufs = [Buf(None) for _ in range(nsub)]
    win_ap = arena.bf16(KC * 1536).rearrange("p (k f) -> p k f", f=1536)
    win_b = Buf(None)
    win_v = dr["ab_w_in"][i].rearrange("(k p) f -> p k f", p=P)
    for g in range(6):
        fetch(win_v[:, :, g * 256:(g + 1) * 256], win_ap[:, :, g * 256:(g + 1) * 256], [win_b], 8, 256)
    wout_ap = arena.bf16(KC * D).rearrange("p (k d) -> p k d", d=D)
    wout_b = Buf(None)
    wout_v = dr["ab_w_out"][i].rearrange("(k p) d -> p k d", p=P)
    for g in range(4):
        fetch(wout_v[:, 2 * g:2 * g + 2, :], wout_ap[:, 2 * g:2 * g + 2, :], [wout_b], 2, 1024)
    pw_ap = arena.bf16(4 * P).rearrange("p (g d) -> p g d", d=P)
    pw_b = Buf(None)
    fetch(dr["pool_w"][i].rearrange("g c d -> c g d"), pw_ap, [pw_b], 4, P)
    sw32 = Buf(arena.f32(4 * P))
    sw3 = sw32.ap.rearrange("p (g s) -> p g s", s=P)
    sem_m = S.new_dma_sem("misc")
    S.dma("sp", lambda e: e.dma_start(out=sw3, in_=dr["sgu_w"][i].rearrange("g t s -> t g s")), sem_m, writes=[sw32])
    swm = Buf(arena.bf16(4 * P))
    swm3 = swm.ap.rearrange("p (g s) -> p g s", s=P)
    tril = cx.cst32.ap[:, 256:384]
    for g in range(4):
        S.op("dve", lambda e, g=g: e.tensor_tensor(out=swm3[:, g, :], in0=sw3[:, g, :], in1=tril, op=ALU.mult),
             reads=[sw32, cx.cst32], writes=[swm])
    wT = Buf(arena.bf16(4 * P))
    wT3 = wT.ap.rearrange("p (g t) -> p g t", t=P)
    bank = cx.next_bank_t()
    pv = bank.ap.bitcast(BF16)[:, 0:4 * P].rearrange("p (g t) -> p g t", t=P)
    S.op("pe", [lambda e, g=g: e.transpose(out=pv[:, g, :], in_=swm3[:, g, :], identity=cx.ident.ap) for g in range(4)],
         reads=[swm, cx.ident], writes=[bank])
    S.op("act", lambda e: e.copy(out=wT3, in_=pv), reads=[bank], writes=[wT])
    psc = Buf(arena.f32(4))
    S.dma("sp", lambda e: e.dma_start(out=psc.ap, in_=dr["pool_scale"][i].rearrange("g d -> d g"),
                                      allow_slow_non_contiguous=True), sem_m, writes=[psc])
    normbc = Buf(arena.f32(512))
    S.dma("sp", lambda e: e.dma_start(out=normbc.ap, in_=flat_bcast(dr["sgu_norm"][i])), sem_m, writes=[normbc])
    bbc = Buf(arena.f32(512))
    S.dma("sp", lambda e: e.dma_start(out=bbc.ap, in_=flat_bcast(dr["sgu_b"][i])), sem_m, writes=[bbc])
    inv0 = Buf(arena.f32(2048))
    S.dma("sp", lambda e: e.dma_start(out=inv0.ap, in_=dr["pcst"][0:1, :].partition_broadcast(P)), sem_m, writes=[inv0])
    inv1 = Buf(arena.f32(2048))
    S.dma("sp", lambda e: e.dma_start(out=inv1.ap, in_=dr["pcst"][1:2, :].partition_broadcast(P)), sem_m, writes=[inv1])
    for _b in (sw32, psc, normbc, bbc, inv0, inv1):
        if _b.w is not None and _b.w[0] is sem_m:
            _b.w = (sem_m, sem_m.v)
    HW = 16
    W = HW + T
    abuf = [Buf(arena.f32(W)) for _ in range(4)]
    sbuf = [Buf(arena.f32(W)) for _ in range(4)]
    for g in range(4):
        S.op("dve", lambda e, g=g: e.memset(abuf[g].ap[:, 0:HW], 0.0), writes=[abuf[g]])
    dT = Buf(arena.bf16(4 * T))
    dT3 = dT.ap.rearrange("p (g t) -> p g t", t=T)
    uT = Buf(arena.f32(4 * T))
    uT3 = uT.ap.rearrange("p (g t) -> p g t", t=T)
    gv = [Buf(arena.f32(512)) for _ in range(2)]
    vn = [Buf(arena.bf16(512)) for _ in range(2)]
    yT = Buf(arena.bf16(8 * T))
    yT3 = yT.ap.rearrange("p (f t) -> p f t", t=T)
    stmp = [Buf(arena.f32(512)) for _ in range(2)]
    ss4 = [Buf(arena.f32(4)) for _ in range(2)]
    sd4 = [Buf(arena.f32(4)) for _ in range(2)]
    rs4 = [Buf(arena.f32(4)) for _ in range(2)]
    S.barrier()
    al = stg_pos
    uT_l = [uT, Buf(arena.t[:, al:al + 4 * T])]
    al += 4 * T
    vn_l = []
    for _p in range(2):
        row = []
        for _c in range(nsub):
            row.append(Buf(arena.t[:, al:al + 256].bitcast(BF16)))
            al += 256
        vn_l.append(row)
    assert al <= stg_pos + 4 * 2048
    ntile = ntok // T
    vi_box = [0]

    def front_ab(tile):
        row0 = tile * T
        uT3 = uT_l[tile % 2].ap.rearrange("p (g t) -> p g t", t=T)
        norm_transpose(S, cx, bufs, h_src, row0, nsub, gpre, xnT_bufs, xnT_ap, T)
        for zc in range(8):
            bank = cx.next_bank_a()
            S.op("pe", [lambda e, k=k, bank=bank, zc=zc: e.matmul(bank.ap, win_ap[:, k, zc * P:(zc + 1) * P], xnT_ap[:, k, :],
                                                                start=(k == 0), stop=(k == KC - 1)) for k in range(KC)],
                 reads=[win_b] + xnT_bufs, writes=[bank])
            if zc < 4:
                S.op("act", lambda e, bank=bank, zc=zc: e.copy(out=abuf[zc].ap[:, HW:W], in_=bank.ap),
                     reads=[bank], writes=[abuf[zc]])
            else:
                S.op("act", lambda e, bank=bank, zc=zc, uT3=uT3: e.activation(out=uT3[:, zc - 4, :], in_=bank.ap, func=AF.Gelu),
                     reads=[bank], writes=[uT_l[tile % 2]])

    def front_c(tile):
        inv = inv0 if tile == 0 else inv1
        inv3 = inv.ap.rearrange("p (g t) -> p g t", t=512)
        for g in range(4):
            src = abuf[g]
            for lv in range(g + 1):
                sh = 1 << lv
                lo = 2 * sh - 1
                dst = sbuf[lv]
                S.op("dve", lambda e, src=src, dst=dst, sh=sh, lo=lo: e.tensor_tensor(
                    out=dst.ap[:, lo:W], in0=src.ap[:, lo:W], in1=src.ap[:, lo - sh:W - sh], op=ALU.add),
                    reads=[src], writes=[dst])
                src = dst
            S.op("dve", lambda e, src=src, g=g, inv3=inv3: e.tensor_tensor(out=src.ap[:, HW:W], in0=src.ap[:, HW:W], in1=inv3[:, g, :], op=ALU.mult),
                 reads=[src, inv], writes=[src])
            S.op("dve", lambda e, src=src, g=g: e.tensor_tensor(out=dT3[:, g, :], in0=src.ap[:, HW:W], in1=abuf[g].ap[:, HW:W], op=ALU.subtract),
                 reads=[src, abuf[g]], writes=[dT])
            S.op("dve", lambda e, g=g: e.tensor_copy(out=abuf[g].ap[:, 0:HW], in_=abuf[g].ap[:, T:W]),
                 reads=[abuf[g]], writes=[abuf[g]])

    def front_v(tile):
        for ch in range(nsub):
            csl = slice(ch * P, (ch + 1) * P)
            bank = cx.next_bank_a()
            S.op("pe", [lambda e, k=k, bank=bank, csl=csl: e.matmul(bank.ap, xnT_ap[:, k, csl], win_ap[:, k, 1024:1536],
                                                                  start=(k == 0), stop=(k == KC - 1)) for k in range(KC)],
                 reads=[win_b, xnT_bufs[ch]], writes=[bank])
            vi = vi_box[0]
            vi_box[0] += 1
            gb, s4, d4, r4, tb = gv[vi % 2], ss4[vi % 2], sd4[vi % 2], rs4[vi % 2], stmp[vi % 2]
            vb = vn_l[tile % 2][ch]
            S.op("act", lambda e, bank=bank, gb=gb: e.activation(out=gb.ap, in_=bank.ap, func=AF.Gelu), reads=[bank], writes=[gb])
            for g in range(4):
                S.op("act", lambda e, g=g, gb=gb, s4=s4, tb=tb: e.activation(out=tb.ap[:, g * P:(g + 1) * P], in_=gb.ap[:, g * P:(g + 1) * P],
                                                                          func=AF.Square, accum_out=s4.ap[:, g:g + 1]),
                     reads=[gb], writes=[tb, s4])
            S.op("act", lambda e, s4=s4, d4=d4: e.activation(out=d4.ap, in_=s4.ap, func=AF.Sqrt, bias=cx.eps_ap, scale=1.0 / P),
                 reads=[s4], writes=[d4])
            S.op("dve", lambda e, d4=d4, r4=r4: e.reciprocal(out=r4.ap, in_=d4.ap), reads=[d4], writes=[r4])
            for g in range(4):
                S.op("dve", lambda e, g=g, gb=gb, vb=vb, r4=r4: e.scalar_tensor_tensor(
                    out=vb.ap[:, g * P:(g + 1) * P], in0=gb.ap[:, g * P:(g + 1) * P], scalar=r4.ap[:, g:g + 1],
                    in1=normbc.ap[:, g * P:(g + 1) * P], op0=ALU.mult, op1=ALU.mult), reads=[gb, r4, normbc], writes=[vb])

    def front_d(tile):
        for g in range(4):
            bank = cx.next_bank_a()
            S.op("pe", lambda e, bank=bank, g=g: e.matmul(bank.ap, pw_ap[:, g, :], dT3[:, g, :], start=True, stop=True),
                 reads=[pw_b, dT], writes=[bank])
            S.op("act", lambda e, bank=bank, g=g: e.activation(out=yT3[:, g, :], in_=bank.ap, func=AF.Copy, scale=psc.ap[:, g:g + 1]),
                 reads=[bank, psc], writes=[yT])

    def back(tile):
        row0 = tile * T
        uTb = uT_l[tile % 2]
        uT3 = uTb.ap.rearrange("p (g t) -> p g t", t=T)
        for ch in range(nsub):
            csl = slice(ch * P, (ch + 1) * P)
            vb = vn_l[tile % 2][ch]
            tb = stmp[ch % 2]
            bank2 = cx.next_bank_a()
            S.op("pe", [lambda e, g=g, bank2=bank2, vb=vb: e.matmul(bank2.ap[:, g * P:(g + 1) * P], vb.ap[:, g * P:(g + 1) * P], wT3[:, g, :],
                                                                  start=True, stop=True) for g in range(4)],
                 reads=[vb, wT], writes=[bank2])
            S.op("dve", lambda e, bank2=bank2, tb=tb: e.tensor_tensor(out=tb.ap, in0=bank2.ap, in1=bbc.ap, op=ALU.add),
                 reads=[bank2, bbc], writes=[tb])
            S.op("dve", lambda e, tb=tb, csl=csl, uT3=uT3: e.tensor_tensor(out=yT3[:, 4:8, csl], in0=tb.ap.rearrange("p (g t) -> p g t", t=P),
                                                                        in1=uT3[:, :, csl], op=ALU.mult), reads=[tb, uTb], writes=[yT])
        for ch in range(nsub):
            b2 = cx.next_bank_pair()
            for dh in range(2):
                S.op("pe", [lambda e, f=f, dh=dh, ch=ch, bk=b2[dh]: e.matmul(bk.ap, yT3[:, f, ch * P:(ch + 1) * P], wout_ap[:, f, dh * 512:(dh + 1) * 512],
                                                                          start=(f == 0), stop=(f == 7)) for f in range(8)],
                     reads=[yT, wout_b], writes=[b2[dh]])
            post_norm_residual(S, cx, b2, cx.pair_ap(b2), gpost, h_src, h_dst, row0 + ch * P, False, bufs)

    front_ab(0)
    front_c(0)
    front_v(0)
    front_d(0)
    for tile in range(ntile):
        if tile + 1 < ntile:
            front_ab(tile + 1)
            front_c(tile + 1)
            front_v(tile + 1)
        back(tile)
        if tile + 1 < ntile:
            front_d(tile + 1)
    S.barrier()


def moba_stage(S, cx, arena, h_src, h_dst, dr, i, layer, ntok, T=512):
    NB = ntok // MOBA_BLOCK
    NH = 8
    scale = float(P) ** -0.5
    qT_d, kT_d, v_d = cx.qT_d, cx.kT_d, cx.v_d
    arena.reset(cx.arena_base)
    wo_ap = arena.bf16(KC * D).rearrange("p (k d) -> p k d", d=D)
    wo_b = Buf(None)
    base2 = arena.pos
    bufs = setup_common(S, cx, arena, T)
    gpre = load_bcast(S, cx, arena, dr["norm_pre"][layer, 1:2, :], "gpre")
    fetch = make_stager(S, cx, arena)
    wo_v = dr["attn_w_o"][i].rearrange("(k p) d -> p k d", p=P)
    for g in range(4):
        fetch(wo_v[:, 2 * g:2 * g + 2, :], wo_ap[:, 2 * g:2 * g + 2, :], [wo_b], 2, 1024)
    nsub = T // P
    xnT_ap = arena.bf16(KC * T).rearrange("p (k t) -> p k t", t=T)
    xnT_bufs = [Buf(None) for _ in range(nsub)]
    wq = [Buf(arena.bf16(KC * 256)) for _ in range(3)]
    wr = [Buf(arena.bf16(KC * 256)) for _ in range(3)]
    for _w in wr:
        S.op("pool", lambda e, _w=_w: e.memset(_w.ap, 0.0), writes=[_w])
    rope = [Buf(arena.f32(2 * T)) for _ in range(2)]
    rope_sems = [S.new_dma_sem(f"rope{k}") for k in range(2)]
    t1 = [Buf(arena.f32(T)) for _ in range(2)]
    t2 = [Buf(arena.f32(T)) for _ in range(2)]
    ot = [Buf(arena.bf16(T)) for _ in range(3)]
    ot_sems = [S.new_dma_sem(f"ot{k}") for k in range(3)]
    vt = [Buf(arena.bf16(256)) for _ in range(3)]
    vt_sems = [S.new_dma_sem(f"vt{k}") for k in range(3)]
    wv_all = dr["attn_w_qkv"][i].rearrange("(k p) f -> p k f", p=P)
    wi = ti = oi = vi = 0
    ntile_a = ntok // T
    tokq, tokr = {}, {}
    wq_sems = [S.new_dma_sem(f"wq{k}") for k in range(3)]
    wr_sems = [S.new_dma_sem(f"wr{k}") for k in range(3)]
    for tile in range(ntile_a):
        row0 = tile * T
        norm_transpose(S, cx, bufs, h_src, row0, nsub, gpre, xnT_bufs, xnT_ap, T)
        rb = rope[tile % 2]
        rb3 = rb.ap[0:32, :].rearrange("p (c t) -> p c t", t=T)
        S.dma("sp", lambda e, rb3=rb3, row0=row0: e.dma_start(out=rb3, in_=dr["rope"][:, :, row0:row0 + T]), rope_sems[tile % 2], writes=[rb])
        for g in range(12):
            wb, wrb = wq[wi % 3], wr[wi % 3]
            wi += 1
            wb3 = wb.ap.rearrange("p (k f) -> p k f", f=256)
            wr4 = wrb.ap.rearrange("p (k h c) -> p k h c", h=2, c=P)

            def extra(sv, sb, wrb=wrb, wr4=wr4, g=g):
                if g >= 8:
                    return
                sv4 = sv.rearrange("p k (h c) -> p k h c", c=P)
                S.op("pool", lambda e: e.tensor_copy(out=wr4[:, :, :, 0:16], in_=sv4[:, :, :, 16:32]), reads=[sb], writes=[wrb])
                S.op("pool", lambda e: e.tensor_copy(out=wr4[:, :, :, 16:32], in_=sv4[:, :, :, 0:16]), reads=[sb], writes=[wrb])
            slot = (wi - 1) % 3
            if tile == 0:
                fetch(wv_all[:, :, g * 256:(g + 1) * 256], wb3, [wb], 8, 256, extra=extra)
                if ntile_a > 1:
                    tokq[g] = S.dma("act", lambda e, wb=wb, g=g: e.dma_start(out=cx.wcq[g], in_=wb.ap), wq_sems[slot], reads=[wb])
                    if g < 8:
                        tokr[g] = S.dma("act", lambda e, wrb=wrb, g=g: e.dma_start(out=cx.wcr[g], in_=wrb.ap), wr_sems[slot], reads=[wrb])
            else:
                S.dma("sp", lambda e, wb=wb, g=g: e.dma_start(out=wb.ap, in_=cx.wcq[g]), wq_sems[slot], writes=[wb], deps=[tokq[g]])
                if g < 8:
                    S.dma("sp", lambda e, wrb=wrb, g=g: e.dma_start(out=wrb.ap, in_=cx.wcr[g]), wr_sems[slot], writes=[wrb], deps=[tokr[g]])
            if g < 8:
                dst_d = qT_d if g < 4 else kT_d
                for hh in range(2):
                    h = (g % 4) * 2 + hh
                    bA = cx.next_bank_a()
                    S.op("pe", [lambda e, k=k, bA=bA, hh=hh, wb3=wb3: e.matmul(bA.ap, wb3[:, k, hh * P:(hh + 1) * P], xnT_ap[:, k, :],
                                                                              start=(k == 0), stop=(k == KC - 1)) for k in range(KC)],
                         reads=[wb] + xnT_bufs, writes=[bA])
                    bB = cx.next_bank_a()
                    S.op("pe", [lambda e, k=k, bB=bB, hh=hh, wr4=wr4: e.matmul(bB.ap, wr4[:, k, hh, :], xnT_ap[:, k, :],
                                                                              start=(k == 0), stop=(k == KC - 1)) for k in range(KC)],
                         reads=[wrb] + xnT_bufs, writes=[bB])
                    a1, a2 = t1[ti % 2], t2[ti % 2]
                    ti += 1
                    ob = ot[oi % 3]
                    osem = ot_sems[oi % 3]
                    oi += 1
                    S.op("dve", lambda e, a1=a1, bA=bA, rb3=rb3: e.tensor_tensor(out=a1.ap[0:32, :], in0=bA.ap[0:32, :], in1=rb3[:, 0, :], op=ALU.mult),
                         reads=[bA, rb], writes=[a1])
                    S.op("dve", lambda e, a2=a2, bB=bB, rb3=rb3: e.tensor_tensor(out=a2.ap[0:32, :], in0=bB.ap[0:32, :], in1=rb3[:, 1, :], op=ALU.mult),
                         reads=[bB, rb], writes=[a2])
                    S.op("dve", lambda e, a1=a1, a2=a2, ob=ob: e.tensor_tensor(out=ob.ap[0:32, :], in0=a1.ap[0:32, :], in1=a2.ap[0:32, :], op=ALU.add),
                         reads=[a1, a2], writes=[ob])
                    S.op("act", lambda e, ob=ob, bA=bA: e.copy(out=ob.ap[32:64, :], in_=bA.ap[32:64, :]), reads=[bA], writes=[ob])
                    S.op("act", lambda e, ob=ob, bA=bA: e.copy(out=ob.ap[64:128, :], in_=bA.ap[64:128, :]), reads=[bA], writes=[ob])
                    S.dma("act", lambda e, ob=ob, h=h, dst_d=dst_d, row0=row0: e.dma_start(out=dst_d[h, :, row0:row0 + T], in_=ob.ap),
                          osem, reads=[ob])
            else:
                for ch in range(nsub):
                    bank = cx.next_bank_a()
                    S.op("pe", [lambda e, k=k, bank=bank, ch=ch, wb3=wb3: e.matmul(bank.ap[:, 0:256], xnT_ap[:, k, ch * P:(ch + 1) * P], wb3[:, k, :],
                                                                                  start=(k == 0), stop=(k == KC - 1)) for k in range(KC)],
                         reads=[wb, xnT_bufs[ch]], writes=[bank])
                    vb = vt[vi % 3]
                    vsem = vt_sems[vi % 3]
                    vi += 1
                    S.op("act", lambda e, vb=vb, bank=bank: e.copy(out=vb.ap, in_=bank.ap[:, 0:256]), reads=[bank], writes=[vb])
                    r0 = row0 + ch * P
                    c0 = (g - 8) * 256
                    S.dma("act", lambda e, vb=vb, r0=r0, c0=c0: e.dma_start(out=v_d[r0:r0 + P, c0:c0 + 256], in_=vb.ap), vsem, reads=[vb])
    S.barrier()
    arena.reset(base2)
    bufs = setup_common(S, cx, arena, T, norm=False)
    gpost = load_bcast(S, cx, arena, dr["norm_post"][layer, 1:2, :], "gpost")
    oT_ap = arena.bf16(NH * ntok).rearrange("p (h t) -> p h t", t=ntok)
    oT_b = [Buf(None) for _ in range(NH)]
    kTs = [Buf(arena.bf16(ntok)) for _ in range(2)]
    qTs = [Buf(arena.bf16(ntok)) for _ in range(2)]
    vhs = [Buf(arena.bf16(ntok)) for _ in range(2)]
    hd_sems = [[S.new_dma_sem(f"hd{a}{k}") for k in range(2)] for a in range(3)]
    NQ = ntok // P - 8
    MbT = [Buf(arena.bf16(ntok)) for _ in range(2)]
    for _m in MbT:
        S.op("dve", lambda e, _m=_m: e.memset(_m.ap, 0.0), writes=[_m])
    km32 = Buf(arena.f32(NB))
    kmb = [Buf(arena.bf16(16)) for _ in range(2)]
    Gall = Buf(arena.f32(max(NQ, 1) * 16))
    m8all = Buf(arena.f32(max(NQ, 1) * 8))
    Mball = Buf(arena.bf16(max(NQ, 1) * 16))
    cmask = Buf(arena.f32(max(NQ, 1) * 16))
    PT = [Buf(arena.bf16(512)) for _ in range(3)]
    rec = [Buf(arena.f32(256)) for _ in range(2)]
    E_bf = Buf(arena.bf16(2048))
    e32 = Buf(arena.f32(2048))
    sem_m = S.new_dma_sem("misc2")
    S.dma("sp", lambda e: e.dma_start(out=e32.ap, in_=dr["cst2"]), sem_m, writes=[e32])
    S.op("dve", lambda e: e.tensor_copy(out=E_bf.ap, in_=e32.ap), reads=[e32], writes=[E_bf])
    sem_m3 = S.new_dma_sem("misc3")
    S.dma("sp", lambda e: e.dma_start(out=cmask.ap, in_=dr["cst3"]), sem_m3, writes=[cmask])
    caus = Buf(arena.bf16(512))
    S.op("dve", lambda e: e.tensor_copy(out=caus.ap, in_=cx.cst32.ap[:, 384:896]), reads=[cx.cst32], writes=[caus])
    ones = Buf(arena.bf16(P))
    S.op("dve", lambda e: e.memset(ones.ap, 1.0), writes=[ones])
    bS = [cx.banks[0], cx.banks[1], cx.banks[2]]
    bG = cx.banks[3]
    bO = [cx.banks[4], cx.banks[5]]
    bL = [cx.banks[6], cx.banks[7]]
    si = pi = gi = 0
    v_view = v_d.rearrange("(t p) f -> p t f", p=P)
    for h in range(NH):
        kb, qb, vb = kTs[h % 2], qTs[h % 2], vhs[h % 2]
        S.dma("sp", lambda e, kb=kb, h=h: e.dma_start(out=kb.ap, in_=kT_d[h]), hd_sems[0][h % 2], writes=[kb])
        S.dma("sp", lambda e, qb=qb, h=h: e.dma_start(out=qb.ap, in_=qT_d[h]), hd_sems[1][h % 2], writes=[qb])
        vb3 = vb.ap.rearrange("p (t d) -> p t d", d=P)
        S.dma("sp", lambda e, vb3=vb3, h=h: e.dma_start(out=vb3, in_=v_view[:, :, h * P:(h + 1) * P]), hd_sems[2][h % 2], writes=[vb])
        mbt = MbT[h % 2]
        kmh = kmb[h % 2]
        S.op("dve", lambda e, kb=kb: e.tensor_reduce(out=km32.ap, in_=kb.ap.rearrange("p (n k) -> p n k", k=MOBA_BLOCK), axis=AX.X, op=ALU.add),
             reads=[kb], writes=[km32])
        S.op("dve", lambda e, kmh=kmh: e.tensor_scalar(out=kmh.ap[:, 0:NB], in0=km32.ap, scalar1=1.0 / MOBA_BLOCK, scalar2=None, op0=ALU.mult),
             reads=[km32], writes=[kmh])
        if NQ > 0:
            S.op("pe", [lambda e, qb=qb, j=j, kmh=kmh: e.matmul(bG.ap[:, j * 16:j * 16 + NB], qb.ap[:, (8 + j) * P:(9 + j) * P], kmh.ap[:, 0:NB],
                                                          start=True, stop=True) for j in range(NQ)],
                 reads=[qb, kmh], writes=[bG])
            G3 = Gall.ap.rearrange("p (j n) -> p j n", n=16)
            m83 = m8all.ap.rearrange("p (j n) -> p j n", n=8)
            Mb3 = Mball.ap.rearrange("p (j n) -> p j n", n=16)
            if NB == 16:
                S.op("dve", lambda e: e.tensor_tensor(out=Gall.ap, in0=bG.ap[:, 0:NQ * 16], in1=cmask.ap, op=ALU.add),
                     reads=[bG, cmask], writes=[Gall])
            else:
                S.op("dve", lambda e: e.tensor_copy(out=Gall.ap, in_=cmask.ap), reads=[cmask], writes=[Gall])
                S.op("dve", lambda e, G3=G3: e.tensor_tensor(out=G3[:, :, 0:NB], in0=bG.ap[:, 0:NQ * 16].rearrange("p (j n) -> p j n", n=16)[:, :, 0:NB],
                                                          in1=cmask.ap.rearrange("p (j n) -> p j n", n=16)[:, :, 0:NB], op=ALU.add),
                     reads=[bG, cmask], writes=[Gall])
            S.op("dve", [lambda e, j=j, G3=G3, m83=m83: e.max(out=m83[:, j, :], in_=G3[:, j, :]) for j in range(NQ)],
                 reads=[Gall], writes=[m8all])
            S.op("dve", lambda e, G3=G3, m83=m83, Mb3=Mb3: e.tensor_tensor(out=Mb3, in0=G3, in1=m83[:, :, 2:3].broadcast_to([P, NQ, 16]), op=ALU.is_lt),
                 reads=[Gall, m8all], writes=[Mball])
        def emit_pv(c, n, pt, kb=kb, qb=qb, vb=vb, vb3=vb3, h=h):
            O, L = bO[c % 2], bL[c % 2]
            qsl = slice(c * 256, (c + 1) * 256)
            fns = []
            for kk in range(2):
                kt = 2 * n + kk
                st_ = (n == 0 and kk == 0)
                sp_ = (n == c and kk == 1)
                fns.append(lambda e, pt=pt, kk=kk, kt=kt, st_=st_, sp_=sp_, O=O, vb3=vb3: e.matmul(
                    O.ap[:, 0:256], vb3[:, kt, :], pt.ap[:, kk * 256:(kk + 1) * 256], start=st_, stop=sp_))
                fns.append(lambda e, pt=pt, kk=kk, st_=st_, sp_=sp_, L=L: e.matmul(
                    L.ap[:, 0:256], ones.ap, pt.ap[:, kk * 256:(kk + 1) * 256], start=st_, stop=sp_))
            if n == 0:
                S.op("pe", fns, reads=[vb, pt, ones], writes=[O, L])
            else:
                S.op("pe", fns, reads=[vb, pt, ones, O, L])
                tok = (S.engsem["pe"], S.engsem["pe"].v)
                O.w = tok
                L.w = tok
            if n == c:
                rc = rec[c % 2]
                S.op("dve", lambda e, rc=rc, L=L: e.reciprocal(out=rc.ap, in_=L.ap[:, 0:256]), reads=[L], writes=[rc])
                S.op("dve", lambda e, rc=rc, O=O, h=h, qsl=qsl: e.tensor_tensor(out=oT_ap[:, h, qsl], in0=O.ap[:, 0:256], in1=rc.ap, op=ALU.mult),
                     reads=[O, rc], writes=[oT_b[h]])

        pend = None
        for c in range(NB):
            qsl = slice(c * 256, (c + 1) * 256)
            if c == 4 and NQ > 0:
                pvb = bG.ap.bitcast(BF16)
                for g0 in range(0, NQ, 8):
                    ng = min(8, NQ - g0)
                    S.op("pe", [lambda e, j=j, g0=g0, Mb3=Mb3, pvb=pvb: e.transpose(out=pvb[0:16, (j - g0) * P:(j - g0 + 1) * P], in_=Mb3[:, j, :],
                                                                                  identity=cx.ident.ap) for j in range(g0, g0 + ng)],
                         reads=[Mball, cx.ident], writes=[bG])
                    S.op("act", lambda e, mbt=mbt, g0=g0, ng=ng, pvb=pvb: e.copy(out=mbt.ap[0:16, (8 + g0) * P:(8 + g0 + ng) * P], in_=pvb[0:16, 0:ng * P]),
                         reads=[bG], writes=[mbt])
            for n in range(c + 1):
                Sb = bS[si % 3]
                si += 1
                fns = []
                for kk in range(2):
                    kt = 2 * n + kk
                    osl = slice(kk * 256, (kk + 1) * 256)
                    masked = (n == c) or (c >= 4)
                    fns.append(lambda e, Sb=Sb, osl=osl, kt=kt, masked=masked, kb=kb, qb=qb, qsl=qsl: e.matmul(
                        Sb.ap[:, osl], kb.ap[:, kt * P:(kt + 1) * P], qb.ap[:, qsl], start=True, stop=not masked))
                    if n == c:
                        fns.append(lambda e, Sb=Sb, osl=osl, kk=kk: e.matmul(
                            Sb.ap[:, osl], cx.ident.ap, caus.ap[:, kk * 256:(kk + 1) * 256], start=False, stop=True))
                    elif c >= 4:
                        fns.append(lambda e, Sb=Sb, osl=osl, n=n, mbt=mbt, qsl=qsl: e.matmul(
                            Sb.ap[:, osl], E_bf.ap[:, n * P:(n + 1) * P], mbt.ap[:, qsl], start=False, stop=True))
                S.op("pe", fns, reads=[kb, qb, cx.ident, caus, E_bf, mbt], writes=[Sb])
                pt = PT[pi % 3]
                pi += 1
                S.op("act", lambda e, pt=pt, Sb=Sb: e.activation(out=pt.ap, in_=Sb.ap, func=AF.Exp, scale=scale), reads=[Sb], writes=[pt])
                if pend is not None:
                    emit_pv(*pend)
                pend = (c, n, pt)
        emit_pv(*pend)
    for ch in range(ntok // P):
        b2 = [cx.banks[0], cx.banks[1]] if ch % 2 == 0 else [cx.banks[2], cx.banks[3]]
        for dh in range(2):
            S.op("pe", [lambda e, hh=hh, dh=dh, ch=ch, bk=b2[dh]: e.matmul(bk.ap, oT_ap[:, hh, ch * P:(ch + 1) * P], wo_ap[:, hh, dh * 512:(dh + 1) * 512],
                                                                        start=(hh == 0), stop=(hh == NH - 1)) for hh in range(NH)],
                 reads=oT_b + [wo_b], writes=[b2[dh]])
        post_norm_residual(S, cx, b2, cx.pair_ap(b2), gpost, h_src, h_dst, ch * P, False, bufs)
    S.barrier()


def stage_inputs(stages):
    need = {"x", "norm_pre", "norm_post", "cst"}
    for st in stages:
        if st[0] == "ffn":
            pre = "ffn1" if st[2] == 0 else "ffn2"
            need |= {pre + "_w_gate", pre + "_w_up", pre + "_w_down"}
        elif st[0] == "poolsgu":
            need |= {"ab_w_in", "pool_w", "pool_scale", "sgu_norm", "sgu_w", "sgu_b", "ab_w_out", "pcst"}
        elif st[0] == "moba":
            need |= {"attn_w_qkv", "attn_w_o", "rope", "cst2", "cst3"}
    return need


def input_shapes(ntok):
    sh = {"x": (ntok, D), "norm_pre": (DEPTH, 3, D), "norm_post": (DEPTH, 3, D), "cst": (P, 1024),
          "ab_w_in": (2, D, 1536), "pool_w": (2, 4, P, P), "pool_scale": (2, 4, P), "sgu_norm": (2, 4, P),
          "sgu_w": (2, 4, P, P), "sgu_b": (2, 4, P), "ab_w_out": (2, D, D), "pcst": (2, 2048),
          "attn_w_qkv": (2, D, 3 * D), "attn_w_o": (2, D, D), "rope": (32, 2, ntok), "cst2": (P, 2048),
          "cst3": (P, max(ntok // P - 8, 1) * 16)}
    for f in ("ffn1", "ffn2"):
        sh[f + "_w_gate"] = (DEPTH, D, DFF)
        sh[f + "_w_up"] = (DEPTH, D, DFF)
        sh[f + "_w_down"] = (DEPTH, DFF, D)
    return sh


def build_program(stages, ntok=SEQ):
    nc = bass.Bass("TRN2", target_bir_lowering=False)
    dr = {}
    shapes = input_shapes(ntok)
    for name in sorted(stage_inputs(stages)):
        dr[name] = nc.dram_tensor(name, list(shapes[name]), F32, kind="ExternalInput").ap()
    out = nc.dram_tensor("out", [ntok, D], F32, kind="ExternalOutput").ap()

    es = ExitStack()
    with es:
        arena_t = es.enter_context(nc.sbuf_tensor("arena", [P, ARENA_COLS], F32))
        psum_t = es.enter_context(nc.psum_tensor("psum", [P, 4096], F32))
        S = Sched(nc, es)
        cx = Ctx()
        if any(st[0] == "moba" for st in stages):
            cx.qT_d = nc.dram_tensor("qT_d", [8, P, ntok], BF16).ap()
            cx.kT_d = nc.dram_tensor("kT_d", [8, P, ntok], BF16).ap()
            cx.v_d = nc.dram_tensor("v_d", [ntok, D], BF16).ap()
            cx.wcq = nc.dram_tensor("wcq", [12, P, 2048], BF16).ap()
            cx.wcr = nc.dram_tensor("wcr", [8, P, 2048], BF16).ap()
        arena = Arena(arena_t, ARENA_COLS)
        cst32 = Buf(arena.f32(1024))
        csem = S.new_dma_sem("cst")
        S.dma("sp", lambda e: e.dma_start(out=cst32.ap, in_=dr["cst"]), csem, writes=[cst32])
        cx.ident = Buf(arena.bf16(P))
        S.op("dve", lambda e: e.tensor_copy(out=cx.ident.ap, in_=cst32.ap[:, 0:P]), reads=[cst32], writes=[cx.ident])
        cx.eps_ap = cst32.ap[:, 128:129]
        cx.eps4_ap = cst32.ap[:, 129:130]
        cx.cst32 = cst32
        cx.arena_base = arena.pos
        cx.banks = [Buf(psum_t[:, i * 512:(i + 1) * 512]) for i in range(8)]
        cx.ba_i = cx.bt_i = cx.bp_i = 0

        def next_bank_a():
            b = cx.banks[cx.ba_i % 4]
            cx.ba_i += 1
            return b

        def next_bank_t():
            b = cx.banks[4 + cx.bt_i % 4]
            cx.bt_i += 1
            return b

        def next_bank_pair():
            k = 4 + 2 * (cx.bp_i % 2)
            cx.bp_i += 1
            return [cx.banks[k], cx.banks[k + 1]]

        def pair_ap(b2):
            k = cx.banks.index(b2[0])
            return psum_t[:, k * 512:(k + 2) * 512]
        cx.next_bank_a, cx.next_bank_t, cx.next_bank_pair, cx.pair_ap = next_bank_a, next_bank_t, next_bank_pair, pair_ap
        cx.hin_sems = [S.new_dma_sem(f"hin{k}") for k in range(2)]
        cx.hep_sems = [S.new_dma_sem(f"hep{k}") for k in range(3)]
        cx.stage_sems = [S.new_dma_sem(f"stg{k}") for k in range(4)]
        cx.wgub_sems = [S.new_dma_sem(f"wgub{k}") for k in range(4)]
        cx.wdb_sems = [S.new_dma_sem(f"wdb{k}") for k in range(JC // 2)]
        cx.wc = [nc.dram_tensor(f"wc_{n_}", [JC // 2, P, 2048], BF16).ap() for n_ in ("g", "u", "d")]

        first = True
        for st in stages:
            h_src = dr["x"] if first else out
            first = False
            if st[0] == "ffn":
                _, layer, which = st
                pre = "ffn1" if which == 0 else "ffn2"
                nk = 0 if which == 0 else 2
                ffn_stage(S, cx, arena, h_src, out,
                          dr[pre + "_w_gate"][layer], dr[pre + "_w_up"][layer], dr[pre + "_w_down"][layer],
                          dr["norm_pre"][layer, nk:nk + 1, :], dr["norm_post"][layer, nk:nk + 1, :], ntok)
            elif st[0] == "poolsgu":
                poolsgu_stage(S, cx, arena, h_src, out, dr, st[1] // 2, st[1], ntok)
            elif st[0] == "moba":
                moba_stage(S, cx, arena, h_src, out, dr, st[1] // 2, st[1], ntok)
            else:
                raise ValueError(st)

        with nc.Block() as block:
            @block.tensor
            def _(e):
                for f in S.q["pe"]:
                    f(e)

            @block.scalar
            def _(e):
                for f in S.q["act"]:
                    f(e)

            @block.vector
            def _(e):
                for f in S.q["dve"]:
                    f(e)

            @block.gpsimd
            def _(e):
                for f in S.q["pool"]:
                    f(e)

            @block.sync
            def _(e):
                for f in S.q["sp"]:
                    f(e)
    return nc


def make_consts(ntok=SEQ):
    c = np.zeros((P, 1024), np.float32)
    c[:, 0:P] = np.eye(P, dtype=np.float32)
    c[:, 128] = EPS
    c[:, 129] = 4 * EPS
    t = np.arange(P)
    c[:, 256:384] = (t[None, :] <= t[:, None]).astype(np.float32)
    q = np.arange(256)
    for kk in range(2):
        c[:, 384 + kk * 256:384 + (kk + 1) * 256] = np.where(q[None, :] >= t[:, None] + 128 * kk, 0.0, NEG)
    c2 = np.zeros((P, 2048), np.float32)
    for n in range(16):
        c2[n, n * P:(n + 1) * P] = NEG
    nq = max(ntok // P - 8, 1)
    c3 = np.zeros((P, nq, 16), np.float32)
    for j in range(nq):
        c3[:, j, (8 + j) // 2:] = -1e30
    pc = np.zeros((2, 4, 512), np.float32)
    tt = np.arange(512)
    for g, w in enumerate(POOL_WINDOWS):
        pc[0, g] = 1.0 / np.minimum(tt + 1, w)
        pc[1, g] = 1.0 / w
    pos = np.arange(ntok, dtype=np.float32)
    inv_freq = (1.0 / (np.float32(500000.0) ** (np.arange(0, 32, 2, dtype=np.float32) / np.float32(32)))).astype(np.float32)
    ang = pos[None, :] * inv_freq[:, None]
    rope = np.zeros((32, 2, ntok), np.float32)
    rope[0:16, 0] = np.cos(ang)
    rope[16:32, 0] = np.cos(ang)
    rope[0:16, 1] = -np.sin(ang)
    rope[16:32, 1] = np.sin(ang)
    return {"cst": c, "cst2": c2, "cst3": c3.reshape(P, nq * 16), "pcst": pc.reshape(2, 2048), "rope": rope}


ALL_STAGES = []
for _l in range(DEPTH):
    ALL_STAGES += [("ffn", _l, 0), ("poolsgu" if _l % 2 == 0 else "moba", _l), ("ffn", _l, 1)]


def kernel(**inputs):
    stages = ALL_STAGES
    nc = build_program(stages, SEQ)
    need = stage_inputs(stages)
    consts = make_consts(SEQ)
    shared = {}
    for name in need:
        if name in consts:
            shared[name] = consts[name]
        elif name != "x":
            shared[name] = np.ascontiguousarray(np.asarray(inputs[name], dtype=np.float32))
    x = np.asarray(inputs["x"], dtype=np.float32)
    in_maps = []
    for c in range(NCORES):
        m = dict(shared)
        m["x"] = np.ascontiguousarray(x[c])
        in_maps.append(m)
    res = run_bass_kernel_spmd(nc, in_maps, core_ids=list(range(NCORES)))
    return np.stack([np.asarray(r["out"]) for r in res.results], axis=0).astype(np.float32)
```
